# Optimizing a Trainium2 kernel written in Bass

```python
import math
import jax, jax.numpy as jnp
from jax import lax
import numpy as np

D_MODEL = 1024
BATCH = 16
SEQ = 2048
DEPTH = 2

EPS = 1e-6
D_FF = 4 * D_MODEL
CONV_CH = D_MODEL // 2
CONV_WIDTH = 31
ATTN_HEAD_DIM = 64
ATTN_HEADS = (D_MODEL // 2) // ATTN_HEAD_DIM
ATTN_WIDTH = ATTN_HEADS * ATTN_HEAD_DIM
MOBA_BLOCK = 256
MOBA_TOP_K = 3
MOBA_Q_CHUNK = 32
AB_IN_WIDTH = 2 * CONV_CH + 3 * ATTN_WIDTH
AB_OUT_WIDTH = CONV_CH + ATTN_WIDTH
LRU_WIDTH = D_MODEL
LRU_HEADS = 8
LRU_BLOCK = LRU_WIDTH // LRU_HEADS
LRU_CONV_WIDTH = 4
LRU_C = 8.0
N_EVEN = (DEPTH + 1) // 2
N_ODD = DEPTH // 2

kernel_name = "hybrid_conformer_moba_rglru_block"


def rmsnorm(x, g):
    xf = x.astype(jnp.float32)
    y = xf * lax.rsqrt(jnp.mean(xf * xf, axis=-1, keepdims=True) + EPS)
    return (y * g.astype(jnp.float32)).astype(x.dtype)


def layernorm(x, g, b):
    xf = x.astype(jnp.float32)
    mu = jnp.mean(xf, axis=-1, keepdims=True)
    var = jnp.mean(jnp.square(xf - mu), axis=-1, keepdims=True)
    y = (xf - mu) * lax.rsqrt(var + EPS)
    return (y * g.astype(jnp.float32) + b.astype(jnp.float32)).astype(x.dtype)


def causal_depthwise_conv(x, w):
    width, ch = w.shape
    return lax.conv_general_dilated(
        x, w[:, None, :].astype(x.dtype), window_strides=(1,),
        padding=[(width - 1, 0)], dimension_numbers=("NWC", "WIO", "NWC"),
        feature_group_count=ch)


def alibi_slopes(n_heads):
    return 2.0 ** (-8.0 * jnp.arange(1, n_heads + 1, dtype=jnp.float32) / n_heads)


def conformer_conv(u_val, u_gate, conv_w, ln_g, ln_b):
    z = u_val * jax.nn.sigmoid(u_gate)
    z = causal_depthwise_conv(z, conv_w)
    z = layernorm(z, ln_g, ln_b)
    return jax.nn.silu(z)


def moba_attention(q, k, v):
    b, s, h, dh = q.shape
    nb = -(-s // MOBA_BLOCK)
    sp = nb * MOBA_BLOCK
    pad = sp - s
    q, k, v = [jnp.pad(t.transpose(0, 2, 1, 3), ((0, 0), (0, 0), (0, pad), (0, 0)))
               for t in (q, k, v)]
    slopes = alibi_slopes(h)[None, :, None, None]
    scale = dh ** -0.5
    n_past = min(MOBA_TOP_K, nb - 1)
    offs = jnp.arange(MOBA_BLOCK)
    kb = k.reshape(b, h, nb, MOBA_BLOCK, dh)
    vb = v.reshape(b, h, nb, MOBA_BLOCK, dh)
    bi = jnp.arange(b)[:, None, None, None]
    hi = jnp.arange(h)[None, :, None, None]

    if n_past > 0:
        kbar = jnp.mean(kb.astype(jnp.float32), axis=3)
        gate = jnp.einsum("bhsd,bhnd->bhsn", q.astype(jnp.float32), kbar)
        qblk = jnp.arange(sp) // MOBA_BLOCK
        past = jnp.arange(nb)[None, :] < qblk[:, None]
        gate = jnp.where(past, gate, -jnp.inf)
        top_val, top_idx = lax.top_k(gate, n_past)
        top_ok = jnp.isfinite(top_val)

    def chunk(c):
        start = c * MOBA_Q_CHUNK
        t = start + jnp.arange(MOBA_Q_CHUNK)
        qc = lax.dynamic_slice_in_dim(q, start, MOBA_Q_CHUNK, axis=2)
        own0 = (start // MOBA_BLOCK) * MOBA_BLOCK
        k_own = lax.dynamic_slice_in_dim(k, own0, MOBA_BLOCK, axis=2)
        v_own = lax.dynamic_slice_in_dim(v, own0, MOBA_BLOCK, axis=2)
        dist_own = (t[:, None] - (own0 + offs)[None, :]).astype(jnp.float32)
        l_own = (jnp.einsum("bhqd,bhkd->bhqk", qc, k_own).astype(jnp.float32) * scale
                 - slopes * dist_own)
        l_own = jnp.where(dist_own >= 0, l_own, -jnp.inf)
        if n_past > 0:
            idx = lax.dynamic_slice_in_dim(top_idx, start, MOBA_Q_CHUNK, axis=2)
            ok = lax.dynamic_slice_in_dim(top_ok, start, MOBA_Q_CHUNK, axis=2)
            k_sel = kb[bi, hi, idx]
            v_sel = vb[bi, hi, idx]
            pos = idx[..., None] * MOBA_BLOCK + offs
            dist = (t[None, None, :, None, None] - pos).astype(jnp.float32)
            l_sel = (jnp.einsum("bhqd,bhqnkd->bhqnk", qc, k_sel).astype(jnp.float32) * scale
                     - slopes[..., None] * dist)
            l_sel = jnp.where(ok[..., None], l_sel, -jnp.inf)
            n_sel = n_past * MOBA_BLOCK
            logits = jnp.concatenate(
                [l_sel.reshape(b, h, MOBA_Q_CHUNK, n_sel), l_own], axis=-1)
            p = jax.nn.softmax(logits, axis=-1).astype(v.dtype)
            p_sel = p[..., :n_sel].reshape(b, h, MOBA_Q_CHUNK, n_past, MOBA_BLOCK)
            p_own = p[..., n_sel:]
            o = (jnp.einsum("bhqnk,bhqnkd->bhqd", p_sel, v_sel)
                 + jnp.einsum("bhqk,bhkd->bhqd", p_own, v_own))
        else:
            p_own = jax.nn.softmax(l_own, axis=-1).astype(v.dtype)
            o = jnp.einsum("bhqk,bhkd->bhqd", p_own, v_own)
        return o

    out = lax.map(chunk, jnp.arange(sp // MOBA_Q_CHUNK))
    out = out.transpose(1, 2, 0, 3, 4).reshape(b, h, sp, dh)[:, :, :s]
    return out.transpose(0, 2, 1, 3)


def rg_lru(x, w_r, b_r, w_i, b_i, lam):
    b, s, w = x.shape
    xb = x.reshape(b, s, LRU_HEADS, LRU_BLOCK)
    r = jax.nn.sigmoid(jnp.einsum("bshi,hij->bshj", xb, w_r) + b_r).reshape(b, s, w)
    i = jax.nn.sigmoid(jnp.einsum("bshi,hij->bshj", xb, w_i) + b_i).reshape(b, s, w)
    log_a = -LRU_C * r.astype(jnp.float32) * jax.nn.softplus(-lam.astype(jnp.float32))
    a = jnp.exp(log_a)
    mult = jnp.sqrt(-jnp.expm1(2.0 * log_a))
    bx = mult * (i * x).astype(jnp.float32)

    def combine(e1, e2):
        a1, b1 = e1
        a2, b2 = e2
        return a1 * a2, a2 * b1 + b2

    _, hseq = lax.associative_scan(combine, (a, bx), axis=1)
    return hseq.astype(x.dtype)


def mixer_ab(h, w_in, conv_w, ln_g, ln_b, w_out):
    b, s, _ = h.shape
    u = h @ w_in
    u_val, u_gate, q, k, v = jnp.split(
        u, np.cumsum([CONV_CH, CONV_CH, ATTN_WIDTH, ATTN_WIDTH]).tolist(), axis=-1)
    ya = conformer_conv(u_val, u_gate, conv_w, ln_g, ln_b)
    shp = (b, s, ATTN_HEADS, ATTN_HEAD_DIM)
    yb = moba_attention(q.reshape(shp), k.reshape(shp), v.reshape(shp)).reshape(b, s, ATTN_WIDTH)
    return jnp.concatenate([ya, yb], axis=-1) @ w_out


def mixer_c(h, w_in, conv_w, conv_b, w_r, b_r, w_i, b_i, lam, w_out):
    u = h @ w_in
    gate, xr = jnp.split(u, 2, axis=-1)
    xr = causal_depthwise_conv(xr, conv_w) + conv_b
    y = rg_lru(xr, w_r, b_r, w_i, b_i, lam) * jax.nn.gelu(gate)
    return y @ w_out


def sq_relu_mlp(h, w_up, w_down):
    return jnp.square(jax.nn.relu(h @ w_up)) @ w_down


def setup_inputs(seed: int = 0) -> dict:
    key = jax.random.key(seed)
    ks = iter(jax.random.split(key, 32))
    f32 = jnp.float32

    def nrm(shape, fan_in):
        return jax.random.normal(next(ks), shape, f32) * (fan_in ** -0.5)

    def gain(shape):
        return 1.0 + 0.02 * jax.random.normal(next(ks), shape, f32)

    def bias(shape):
        return 0.01 * jax.random.normal(next(ks), shape, f32)

    x = jax.random.normal(next(ks), (BATCH, SEQ, D_MODEL), f32)
    a_c = jax.random.uniform(next(ks), (N_ODD, LRU_WIDTH), f32, 0.9, 0.999)
    a_base = a_c ** (1.0 / LRU_C)
    lam = jnp.log(a_base) - jnp.log1p(-a_base)
    return {
        "x": x,
        "mix_norm": gain((DEPTH, D_MODEL)),
        "mlp_norm": gain((DEPTH, D_MODEL)),
        "w_up": nrm((DEPTH, D_MODEL, D_FF), D_MODEL),
        "w_down": nrm((DEPTH, D_FF, D_MODEL), D_FF),
        "ab_w_in": nrm((N_EVEN, D_MODEL, AB_IN_WIDTH), D_MODEL),
        "ab_conv_w": nrm((N_EVEN, CONV_WIDTH, CONV_CH), CONV_WIDTH),
        "ab_ln_g": gain((N_EVEN, CONV_CH)),
        "ab_ln_b": bias((N_EVEN, CONV_CH)),
        "ab_w_out": nrm((N_EVEN, AB_OUT_WIDTH, D_MODEL), AB_OUT_WIDTH),
        "c_w_in": nrm((N_ODD, D_MODEL, 2 * LRU_WIDTH), D_MODEL),
        "c_conv_w": nrm((N_ODD, LRU_CONV_WIDTH, LRU_WIDTH), LRU_CONV_WIDTH),
        "c_conv_b": bias((N_ODD, LRU_WIDTH)),
        "c_w_r": nrm((N_ODD, LRU_HEADS, LRU_BLOCK, LRU_BLOCK), LRU_BLOCK),
        "c_b_r": bias((N_ODD, LRU_HEADS, LRU_BLOCK)),
        "c_w_i": nrm((N_ODD, LRU_HEADS, LRU_BLOCK, LRU_BLOCK), LRU_BLOCK),
        "c_b_i": bias((N_ODD, LRU_HEADS, LRU_BLOCK)),
        "c_lambda": lam,
        "c_w_out": nrm((N_ODD, LRU_WIDTH, D_MODEL), LRU_WIDTH),
        "final_norm": gain((D_MODEL,)),
    }


def reference(x, mix_norm, mlp_norm, w_up, w_down, ab_w_in, ab_conv_w, ab_ln_g, ab_ln_b,
              ab_w_out, c_w_in, c_conv_w, c_conv_b, c_w_r, c_b_r, c_w_i, c_b_i, c_lambda,
              c_w_out, final_norm):
    for layer in range(DEPTH):
        j = layer // 2
        h = rmsnorm(x, mix_norm[layer])
        if layer % 2 == 0:
            y = mixer_ab(h, ab_w_in[j], ab_conv_w[j], ab_ln_g[j], ab_ln_b[j], ab_w_out[j])
        else:
            y = mixer_c(h, c_w_in[j], c_conv_w[j], c_conv_b[j], c_w_r[j], c_b_r[j],
                        c_w_i[j], c_b_i[j], c_lambda[j], c_w_out[j])
        x = x + y
        x = x + sq_relu_mlp(rmsnorm(x, mlp_norm[layer]), w_up[layer], w_down[layer])
    return rmsnorm(x, final_norm)
```

```python
import contextlib
import numpy as np
import concourse.bass as bass
import concourse.mybir as mybir
from concourse.bass_utils import run_bass_kernel_spmd

F32 = mybir.dt.float32
BF16 = mybir.dt.bfloat16
ALU = mybir.AluOpType
AF = mybir.ActivationFunctionType
AX = mybir.AxisListType

ENGS = ["pe", "act", "dve", "pool", "sp"]
_DT_SIZE = {"float32": 4, "bfloat16": 2, "float16": 2, "int32": 4, "uint32": 4,
            "uint8": 1, "int8": 1, "uint16": 2, "int16": 2, "float32r": 4}


def _dsize(dt):
    return _DT_SIZE[str(dt).split(".")[-1]]


def _is_ap(v):
    return hasattr(v, "tensor") and hasattr(v, "ap") and hasattr(v, "offset")


def _footprint(ap):
    tn = type(ap.tensor).__name__
    if tn.startswith("DRam"):
        return None
    dims = ap.ap
    es = _dsize(ap.dtype)
    pstride = dims[0][0]
    off = int(ap.offset)
    if pstride > 0:
        p0 = off // pstride
        foff = off % pstride
    else:
        p0 = 0
        foff = off
    p1 = p0 + dims[0][1]
    free = [(abs(s), n) for (s, n) in dims[1:] if s != 0 and n > 1]
    if not free:
        if tn.startswith("PSum"):
            b0 = (foff * es // 2048) * 2048
            return (ap.tensor.name, p0, p1, [(b0, b0 + 2048)])
        return (ap.tensor.name, p0, p1, [(foff * es, (foff + 1) * es)])
    free.sort()
    s0, n0 = free[0]
    if s0 == 1:
        run = n0
        rest = free[1:]
        while rest and rest[0][0] == run:
            run *= rest[0][1]
            rest = rest[1:]
    else:
        run = 1
        rest = free
    cnt = 1
    for s, n in rest:
        cnt *= n
    if cnt > 64:
        span = run + sum(s * (n - 1) for s, n in rest)
        lo, hi = foff * es, (foff + span) * es
        if tn.startswith("PSum"):
            lo, hi = (lo // 2048) * 2048, ((hi + 2047) // 2048) * 2048
        return (ap.tensor.name, p0, p1, [(lo, hi)])
    starts = [foff]
    for s, n in rest:
        starts = [b + i * s for b in starts for i in range(n)]
    ivs = sorted((b * es, (b + run) * es) for b in starts)
    if tn.startswith("PSum"):
        ivs = sorted(set(((lo // 2048) * 2048, ((hi + 2047) // 2048) * 2048) for lo, hi in ivs))
    out = [ivs[0]]
    for lo, hi in ivs[1:]:
        if lo <= out[-1][1]:
            out[-1] = (out[-1][0], max(hi, out[-1][1]))
        else:
            out.append((lo, hi))
    return (ap.tensor.name, p0, p1, out)


def _ov(a, b):
    if a[0] >= b[1] or b[0] >= a[1]:
        return False
    for lo, hi in a[2]:
        for lo2, hi2 in b[2]:
            if lo < hi2 and lo2 < hi:
                return True
    return False


def _covers(w, o):
    if not (w[0] <= o[0] and o[1] <= w[1]):
        return False
    for lo, hi in o[2]:
        ok = False
        for lo2, hi2 in w[2]:
            if lo2 <= lo and hi <= hi2:
                ok = True
                break
        if not ok:
            return False
    return True


class Inst:
    __slots__ = ("eng", "fn", "deps", "is_dma", "key", "kord", "ordn", "signal", "sigval", "phase")


class Prog:
    def __init__(self, nc):
        self.nc = nc
        self.insts = {e: [] for e in ENGS}
        self.acc = {}
        self.key_count = {}
        self.n = 0
        self.phase = ""

    def op(self, eng, method, outs=("out", "accum_out"), extra_reads=(), dma_key=None, after=(), **kw):
        reads, writes = [], []
        for k, v in kw.items():
            if _is_ap(v):
                fp = _footprint(v)
                if fp is None:
                    continue
                (writes if k in outs else reads).append(fp)
        for v in extra_reads:
            fp = _footprint(v)
            if fp is not None:
                reads.append(fp)
        ins = Inst()
        ins.eng = eng
        ins.phase = self.phase
        ins.fn = (method, kw)
        ins.is_dma = method == "dma_start"
        ins.signal = False
        ins.sigval = 0
        ins.ordn = len(self.insts[eng])
        if ins.is_dma:
            assert dma_key is not None
            c = self.key_count.get(dma_key, 0) + 1
            self.key_count[dma_key] = c
            ins.key = dma_key
            ins.kord = c
            token = ("D", dma_key, c)
        else:
            ins.key = None
            ins.kord = 0
            token = ("E", eng, ins.ordn)
        deps = {}

        def add_dep(tok):
            if tok[0] == "E":
                if tok[1] == "pe" and eng == "pe" and not ins.is_dma:
                    return
                k = ("E", tok[1])
            else:
                k = ("D", tok[1])
            if deps.get(k, -1) < tok[2]:
                deps[k] = tok[2]

        for a_ in after:
            add_dep(("D", a_.key, a_.kord) if a_.is_dma else ("E", a_.eng, a_.ordn))
        for (name, p0, p1, ivs) in reads:
            fpr = (p0, p1, ivs)
            for rec in self.acc.get(name, ()):
                if rec[0] and _ov(fpr, rec[2]):
                    add_dep(rec[1])
        for (name, p0, p1, ivs) in writes:
            fpr = (p0, p1, ivs)
            for rec in self.acc.get(name, ()):
                if _ov(fpr, rec[2]):
                    add_dep(rec[1])
        ins.deps = deps
        for (name, p0, p1, ivs) in writes:
            fpr = (p0, p1, ivs)
            lst = self.acc.setdefault(name, [])
            lst[:] = [r for r in lst if not _covers(fpr, r[2])]
            lst.append((True, token, fpr))
        for (name, p0, p1, ivs) in reads:
            fpr = (p0, p1, ivs)
            lst = self.acc.setdefault(name, [])
            if token[0] == "E":
                lst[:] = [r for r in lst if not ((not r[0]) and r[1][0] == "E" and r[1][1] == eng
                                                 and r[2] == fpr)]
            lst.append((False, token, fpr))
        self.insts[eng].append(ins)
        self.n += 1
        return ins

    def emit(self, final_wait_keys=()):
        nc = self.nc
        for e in ENGS:
            for ins in self.insts[e]:
                for k, v in ins.deps.items():
                    if k[0] == "E":
                        self.insts[k[1]][v].signal = True
        for e in ENGS:
            c = 0
            for ins in self.insts[e]:
                if ins.signal and not ins.is_dma:
                    c += 1
                ins.sigval = c
        self.sig_counts = {e: (self.insts[e][-1].sigval if self.insts[e] else 0) for e in ENGS}
        with contextlib.ExitStack() as st:
            esem = {e: st.enter_context(nc.semaphore("s_" + e)) for e in ENGS}
            ksem = {k: st.enter_context(nc.semaphore("k_%s" % str(k))) for k in self.key_count}
            block = st.enter_context(nc.Block())
            prog = self

            def run(e, engobj):
                known = {}
                for ins in prog.insts[e]:
                    for k, v in ins.deps.items():
                        if k[0] == "E":
                            sem = esem[k[1]]
                            val = prog.insts[k[1]][v].sigval
                        else:
                            sem = ksem[k[1]]
                            val = 16 * v
                        if known.get(k, 0) < val:
                            engobj.wait_ge(sem, val)
                            known[k] = val
                    method, kw = ins.fn
                    bi = getattr(engobj, method)(**kw)
                    if ins.is_dma:
                        bi.then_inc(ksem[ins.key], 16)
                    elif ins.signal:
                        bi.then_inc(esem[e], 1)
                if e == "sp":
                    for k in final_wait_keys:
                        engobj.wait_ge(ksem[k], 16 * prog.key_count[k])

            @block.tensor
            def _(eng):
                run("pe", eng)

            @block.scalar
            def _(eng):
                run("act", eng)

            @block.vector
            def _(eng):
                run("dve", eng)

            @block.gpsimd
            def _(eng):
                run("pool", eng)

            @block.sync
            def _(eng):
                run("sp", eng)


T = 2048
D = 1024
KC = 8
TT = 512
NT = 4
EPS = 1e-6
NEG = -1.0e30
N_CORES = 8
SEQ_PER_CORE = 2

V_MIX = 0
V_MLP = 16
V_FIN = 32
V_LNG = 40
V_LNB = 44
V_CW = 48
V_CCW = 172
V_CCB = 204
V_BR = 212
V_BI = 220
V_LAM = 228
NV = 236
C_ID = 0
C_BIAS = 128
C_M05 = 288
C_P05 = 289
NCST = 290

ARENA_BYTES = 107008
KB_ = 1024
GELU_C = 0.044715
GELU_S = 0.7978845608028654
USE_POOL_POW = False
ZIP_M1 = True


def build(n_seq=SEQ_PER_CORE, stop_stage=5):
    nc = bass.Bass("TRN2", target_bir_lowering=False)
    dr = {}

    def din(name, shape):
        dr[name] = nc.dram_tensor(name, list(shape), F32, kind="ExternalInput").ap()
        return dr[name]

    x_in = din("x", [n_seq, T, D])
    vecs_in = din("vecs", [128, NV])
    cst_in = din("consts", [128, NCST])
    tri_in = din("tri", [128, 128])
    w_up = din("w_up", [2, D, 4 * D])
    w_down = din("w_down", [2, 4 * D, D])
    ab_w_in = din("ab_w_in", [D, 2560])
    ab_w_out = din("ab_w_out", [D, D])
    c_w_in = din("c_w_in", [D, 2 * D])
    c_w_out = din("c_w_out", [D, D])
    c_w_r = din("c_w_r", [8, 128, 128])
    c_w_i = din("c_w_i", [8, 128, 128])
    y_out = nc.dram_tensor("y", [n_seq, T, D], F32, kind="ExternalOutput").ap()
    cw_scr = nc.dram_tensor("cw_scr", [8, 128, 8, 256], BF16).ap()

    with contextlib.ExitStack() as st:
        def sb(name, shape, dt):
            return st.enter_context(nc.sbuf_tensor(name, shape, dt))

        X = sb("X", [128, KC, T], F32)
        HB = sb("HB", [128, KC, T], BF16)
        VEC = sb("VEC", [128, NV], F32)
        CST = sb("CST", [128, NCST], F32)
        IDB = sb("IDB", [128, 128], BF16)
        TRIB = sb("TRIB", [128, 128], BF16)
        ONE1024 = sb("ONE1024", [128, 128], BF16)
        ONE512 = sb("ONE512", [128, 128], BF16)
        HV = sb("HV", [128, 32], F32)
        NSQ = sb("NSQ", [128, 2, TT], BF16)
        NRS = sb("NRS", [128, TT], F32)
        AR = sb("AR", [128, ARENA_BYTES // 2], BF16)
        PSA = st.enter_context(nc.psum_tensor("PSA", [128, 8, 512], F32))

        P = Prog(nc)
        op = P.op

        def PS(i):
            return PSA[:, i, :]

        def PSR(i, n):
            return PSA[:, i:i + n, :].rearrange("p a b -> p (a b)")

        def vw(off, shape, dt):
            es = 4 if dt == F32 else 2
            n = int(np.prod(shape))
            assert off % 4 == 0 and off + n * es <= ARENA_BYTES, (off, shape)
            ap = AR[:, off // 2: off // 2 + n * es // 2]
            if dt == F32:
                ap = ap.bitcast(F32)
            if len(shape) == 2:
                ap = ap.rearrange("p (a b) -> p a b", a=shape[0])
            elif len(shape) == 3:
                ap = ap.rearrange("p (a b c) -> p a b c", a=shape[0], b=shape[1])
            elif len(shape) == 4:
                ap = ap.rearrange("p (a b c d) -> p a b c d", a=shape[0], b=shape[1], c=shape[2])
            return ap

        cnt = [0]
        rl_inst = {}
        from collections import deque
        side = deque()

        def relayout_cw():
            for c in range(KC):
                lst = []
                for part in range(2):
                    lst.append(op("pool", "dma_start", dma_key=("rl", c, part),
                                  out=cw_scr[c][:, :, part * 128:(part + 1) * 128],
                                  in_=c_w_in[:, part * D + c * 128: part * D + (c + 1) * 128]
                                  .rearrange("(kc p) m -> p kc m", p=128)))
                rl_inst[c] = lst

        def evac(out, in_):
            cnt[0] += 1
            if cnt[0] % 2:
                op("act", "activation", out=out, in_=in_, func=AF.Copy)
            else:
                op("dve", "tensor_copy", out=out, in_=in_)

        M05 = CST[:, C_M05:C_M05 + 1]
        P05 = CST[:, C_P05:C_P05 + 1]
        IDF = CST[:, C_ID:C_ID + 128]

        P.phase = "setup"
        op("sp", "dma_start", dma_key="vec", out=VEC[:], in_=vecs_in)
        op("sp", "dma_start", dma_key="cst", out=CST[:], in_=cst_in)
        op("pool", "dma_start", dma_key="tri", out=TRIB[:], in_=tri_in)
        op("dve", "tensor_copy", out=IDB[:], in_=CST[:, C_ID:C_ID + 128])
        op("dve", "memset", outs=("ap",), ap=ONE1024[:], constant=1.0 / 1024)
        op("dve", "memset", outs=("ap",), ap=ONE512[:], constant=1.0 / 512)
        op("act", "activation", out=HV[:, 24:32], in_=VEC[:, V_LAM:V_LAM + 8], func=AF.Exp, scale=-1.0)
        op("act", "activation", out=HV[:, 24:32], in_=HV[:, 24:32], func=AF.Ln, bias=1.0)
        op("dve", "tensor_scalar", out=HV[:, 16:24], in0=HV[:, 24:32], scalar1=-4.0, scalar2=None, op0=ALU.mult)
        op("dve", "tensor_scalar", out=HV[:, 0:8], in0=VEC[:, V_BR:V_BR + 8], scalar1=0.5, scalar2=None, op0=ALU.mult)
        op("dve", "tensor_scalar", out=HV[:, 8:16], in0=VEC[:, V_BI:V_BI + 8], scalar1=0.5, scalar2=None, op0=ALU.mult)

        def load_w(dst, src, key, nk=KC):
            for kc in range(nk):
                op("pool", "dma_start", dma_key=(key, kc), out=dst[:, kc, :],
                   in_=src[kc * 128:(kc + 1) * 128, :])

        def norm_tile(gcol, dst, tt):
            ph = P.phase
            P.phase = "norm"
            ts = slice(tt * TT, (tt + 1) * TT)
            for c in range(KC):
                op("act", "activation", out=NSQ[:, c % 2, :], in_=X[:, c, ts], func=AF.Square)
                op("pe", "matmul", out=PS(7), lhsT=ONE1024[:], rhs=NSQ[:, c % 2, :],
                   start=(c == 0), stop=(c == KC - 1))
            op("act", "activation", out=NRS[:], in_=PS(7), func=AF.Sqrt, bias=EPS)
            op("dve", "reciprocal", out=NRS[:], in_=NRS[:])
            for c in range(KC):
                op("dve", "scalar_tensor_tensor", out=dst(c, tt), in0=X[:, c, ts],
                   scalar=VEC[:, gcol + c:gcol + c + 1], in1=NRS[:], op0=ALU.mult, op1=ALU.mult)
            P.phase = ph

        def hb_dst(c, tt):
            return HB[:, c, tt * TT:(tt + 1) * TT]

        def mlp(l, after_tile):
            WS = [vw(i * 16 * KB_, [KC, 1024], BF16) for i in range(4)]
            Hb = [vw(64 * KB_ + i * 8 * KB_, [KC, TT], BF16) for i in range(2)]
            RL = [vw(80 * KB_ + i * 2 * KB_, [TT], F32) for i in range(2)]
            k = 0
            for g in range(4):
                P.phase = "mlp%d.g%d" % (l, g)
                WU = WS[(2 * g) % 4]
                WD = WS[(2 * g + 1) % 4]
                load_w(WU, w_up[l][:, g * 1024:(g + 1) * 1024], "ws%d" % ((2 * g) % 4))
                load_w(WD, w_down[l][g * 1024:(g + 1) * 1024, :], "ws%d" % ((2 * g + 1) % 4))
                for tt in range(NT):
                    ts = slice(tt * TT, (tt + 1) * TT)
                    H = Hb[tt % 2]
                    for hc in range(KC):
                        pb = k % 3
                        k += 1
                        for kc in range(KC):
                            op("pe", "matmul", out=PS(pb), lhsT=WU[:, kc, hc * 128:(hc + 1) * 128],
                               rhs=HB[:, kc, ts], start=(kc == 0), stop=(kc == KC - 1))
                        op("act", "activation", out=RL[hc % 2], in_=PS(pb), func=AF.Relu)
                        op("act", "activation", out=H[:, hc, :], in_=RL[hc % 2], func=AF.Square)
                        if side:
                            side.popleft()()
                    for m in range(KC):
                        pb = 3 + (k % 3)
                        k += 1
                        for hc in range(KC):
                            op("pe", "matmul", out=PS(pb), lhsT=WD[:, hc, m * 128:(m + 1) * 128],
                               rhs=H[:, hc, :], start=(hc == 0), stop=(hc == KC - 1))
                        op("dve", "tensor_tensor", out=X[:, m, ts], in0=PS(pb), in1=X[:, m, ts], op=ALU.add)
                        if side:
                            side.popleft()()
                    if g == 3:
                        after_tile(tt)
            while side:
                side.popleft()()

        def out_proj(w_src, YC, wb_off, after_tile):
            WB = [vw(wb_off + i * 8 * KB_, [KC, 512], BF16) for i in range(2)]
            k = 0
            P.phase = P.phase.split(".")[0] + ".oproj"
            for hh in range(2):
                load_w(WB[hh], w_src[:, hh * 512:(hh + 1) * 512], "wo%d" % hh)
            for hh in range(2):
                for tt in range(NT):
                    ts = slice(tt * TT, (tt + 1) * TT)
                    for m in range(4):
                        pb = k % 4
                        k += 1
                        for kc in range(KC):
                            op("pe", "matmul", out=PS(pb), lhsT=WB[hh][:, kc, m * 128:(m + 1) * 128],
                               rhs=YC(kc, ts), start=(kc == 0), stop=(kc == KC - 1))
                        mm = hh * 4 + m
                        op("dve", "tensor_tensor", out=X[:, mm, ts], in0=PS(pb), in1=X[:, mm, ts], op=ALU.add)
                    if hh == 1:
                        after_tile(tt)

        def load_x(s, after_tile):
            P.phase = "load"
            XS = [vw(i * 4 * KB_, [D], F32) for i in range(3)]
            for t16 in range(16):
                xs = XS[t16 % 3]
                op("sp", "dma_start", dma_key=("xs", t16 % 3), out=xs,
                   in_=x_in[s, t16 * 128:(t16 + 1) * 128, :])
                for half in range(2):
                    pb = (t16 * 2 + half) % 4
                    for cc in range(4):
                        c = half * 4 + cc
                        op("pe", "transpose", out=PS(pb)[:, cc * 128:(cc + 1) * 128],
                           in_=xs[:, c * 128:(c + 1) * 128], identity=IDF)
                    evac(X[:, half * 4:half * 4 + 4, t16 * 128:(t16 + 1) * 128],
                         PS(pb).rearrange("p (a b) -> p a b", a=4))
                if t16 % 4 == 3:
                    after_tile(t16 // 4)

        def preload_tile(s, tt):
            XS = [vw(i * 4 * KB_, [D], F32) for i in range(3)]

            def job_dma(t16):
                def run_():
                    ph2 = P.phase
                    P.phase = "preload"
                    op("sp", "dma_start", dma_key=("xs", t16 % 3), out=XS[t16 % 3],
                       in_=x_in[s, t16 * 128:(t16 + 1) * 128, :])
                    P.phase = ph2
                return run_

            def job_tr(t16, half):
                def run_():
                    ph2 = P.phase
                    P.phase = "preload"
                    xs = XS[t16 % 3]
                    for cc in range(4):
                        c = half * 4 + cc
                        op("pe", "transpose", out=PS(6)[:, cc * 128:(cc + 1) * 128],
                           in_=xs[:, c * 128:(c + 1) * 128], identity=IDF)
                    evac(X[:, half * 4:half * 4 + 4, t16 * 128:(t16 + 1) * 128],
                         PS(6).rearrange("p (a b) -> p a b", a=4))
                    P.phase = ph2
                return run_

            for j in range(4):
                t16 = tt * 4 + j
                side.append(job_dma(t16))
                side.append(job_tr(t16, 0))
                side.append(job_tr(t16, 1))
            side.append(lambda: norm_tile(V_MIX + 0, hb_dst, tt))

        def store_tile(s, tt, do_norm, deferred=False):
            while side:
                side.popleft()()
            ph = P.phase
            P.phase = "store"
            XN = vw(84 * KB_, [KC, TT], F32)
            OS = vw(100 * KB_, [D], F32)
            if do_norm:
                norm_tile(V_FIN, lambda c, t_: XN[:, c, :], tt)
            P.phase = ph

            def job(j, half):
                def run_():
                    ph2 = P.phase
                    P.phase = "store"
                    t16 = tt * 4 + j
                    for cc in range(4):
                        c = half * 4 + cc
                        src = XN[:, c, j * 128:(j + 1) * 128] if do_norm else \
                            X[:, c, t16 * 128:(t16 + 1) * 128]
                        op("pe", "transpose", out=PS(6)[:, cc * 128:(cc + 1) * 128],
                           in_=src, identity=IDF)
                    evac(OS[:, half * 512:(half + 1) * 512], PS(6))
                    if half == 1:
                        op("sp", "dma_start", dma_key=("os", 0),
                           out=y_out[s, t16 * 128:(t16 + 1) * 128, :], in_=OS)
                    P.phase = ph2
                return run_
            for j in range(4):
                for half in range(2):
                    if deferred:
                        side.append(job(j, half))
                    else:
                        job(j, half)()

        def mixer0(after_tile):
            S0 = 0
            ZW = 2080
            Zo = 16128
            Z = vw(Zo, [4, ZW], BF16)
            WB = [vw(32 * KB_ + i * 8 * KB_, [KC, 512], BF16) for i in range(2)]
            DG = vw(48 * KB_, [31, 128], BF16)
            Qo = 56 * KB_
            Q = vw(Qo, [4, T], BF16)
            K = vw(Qo + 16 * KB_, [4, T], BF16)
            V = vw(Qo + 32 * KB_, [16, 8, 65], BF16)
            P.phase = "m0.win"
            op("dve", "memset", outs=("ap",), ap=Z[:, :, 0:30], constant=0.0)
            op("dve", "memset", outs=("ap",), ap=V[:, :, :, 64:65], constant=1.0)
            groups = [("gate", 512), ("val", 0), ("q", 1024), ("k", 1536), ("v", 2048)]
            k = 0
            for gi, (gname, col0) in enumerate(groups):
                W = WB[gi % 2]
                load_w(W, ab_w_in[:, col0:col0 + 512], "wb%d" % (gi % 2))
                if gname == "v":
                    for t16 in range(16):
                        pb = k % 4
                        k += 1
                        for kc in range(KC):
                            op("pe", "matmul", out=PS(pb), lhsT=HB[:, kc, t16 * 128:(t16 + 1) * 128],
                               rhs=W[:, kc, :], start=(kc == 0), stop=(kc == KC - 1))
                        evac(V[:, t16, :, 0:64], PS(pb).rearrange("p (h d) -> p h d", h=8))
                    continue
                for tt in range(NT):
                    ts = slice(tt * TT, (tt + 1) * TT)
                    zs = slice(30 + tt * TT, 30 + (tt + 1) * TT)
                    for m in range(4):
                        pb = k % 4
                        k += 1
                        for kc in range(KC):
                            op("pe", "matmul", out=PS(pb), lhsT=W[:, kc, m * 128:(m + 1) * 128],
                               rhs=HB[:, kc, ts], start=(kc == 0), stop=(kc == KC - 1))
                        if gname == "gate":
                            op("act", "activation", out=Z[:, m, zs], in_=PS(pb), func=AF.Sigmoid)
                        elif gname == "val":
                            op("dve", "tensor_tensor", out=Z[:, m, zs], in0=PS(pb), in1=Z[:, m, zs],
                               op=ALU.mult)
                        elif gname == "q":
                            evac(Q[:, m, ts], PS(pb))
                        else:
                            evac(K[:, m, ts], PS(pb))
            P.phase = "m0.conv"
            if not rl_inst:
                relayout_cw()
            for c in range(4):
                for j in range(31):
                    col = V_CW + j * 4 + c
                    op("dve", "tensor_scalar", out=DG[:, j, :], in0=IDB[:],
                       scalar1=VEC[:, col:col + 1], scalar2=None, op0=ALU.mult)
                for tt in (3, 2, 1, 0):
                    pb = k % 4
                    k += 1
                    for j in range(31):
                        op("pe", "matmul", out=PS(pb), lhsT=DG[:, j, :],
                           rhs=Z[:, c, tt * TT + j: tt * TT + j + TT], start=(j == 0), stop=(j == 30))
                    op("act", "activation", out=Z[:, c, 30 + tt * TT: 30 + (tt + 1) * TT], in_=PS(pb),
                       func=AF.Copy)
            P.phase = "m0.ln"
            SQ = vw(S0, [4, TT], BF16)
            MEAN = vw(S0 + 4 * KB_, [TT], F32)
            VAR = vw(S0 + 6 * KB_, [TT], F32)
            TM = [vw(S0 + 8 * KB_ + i * 2 * KB_, [TT], F32) for i in range(2)]
            for tt in range(NT):
                zs = slice(30 + tt * TT, 30 + (tt + 1) * TT)
                ts = slice(tt * TT, (tt + 1) * TT)
                for c in range(4):
                    op("act", "activation", out=SQ[:, c, :], in_=Z[:, c, zs], func=AF.Square)
                for c in range(4):
                    op("pe", "matmul", out=PS(4), lhsT=ONE512[:], rhs=Z[:, c, zs], start=(c == 0), stop=(c == 3))
                for c in range(4):
                    op("pe", "matmul", out=PS(5), lhsT=ONE512[:], rhs=SQ[:, c, :], start=(c == 0), stop=(c == 3))
                op("act", "activation", out=MEAN, in_=PS(4), func=AF.Copy)
                op("dve", "tensor_tensor", out=VAR, in0=MEAN, in1=MEAN, op=ALU.mult)
                op("dve", "tensor_tensor", out=VAR, in0=PS(5), in1=VAR, op=ALU.subtract)
                op("act", "activation", out=VAR, in_=VAR, func=AF.Sqrt, bias=EPS)
                op("dve", "reciprocal", out=VAR, in_=VAR)
                for c in range(4):
                    tm = TM[c % 2]
                    op("dve", "tensor_tensor", out=tm, in0=Z[:, c, zs], in1=MEAN, op=ALU.subtract)
                    op("dve", "tensor_tensor", out=tm, in0=tm, in1=VAR, op=ALU.mult)
                    op("act", "activation", out=HB[:, c, ts], in_=tm, func=AF.Silu,
                       scale=VEC[:, V_LNG + c:V_LNG + c + 1], bias=VEC[:, V_LNB + c:V_LNB + c + 1])
            P.phase = "m0.attn"
            A0 = S0
            KBF = vw(A0, [4, 8], F32)
            KBB = vw(A0 + 128, [4, 8], BF16)
            GS = vw(A0 + 256, [8, 8], F32)
            M8 = vw(A0 + 512, [8, 8], F32)
            RCP = vw(A0 + 768, [4], F32)
            SEL = vw(A0 + 1 * KB_, [16, 8, 8], F32)
            PT = [vw(A0 + 5 * KB_ + i * KB_, [TT], BF16) for i in range(4)]
            ACC = [vw(A0 + 9 * KB_ + i * 1536, [4, 65], F32) for i in range(2)]
            OT = vw(Zo, [16, 512], BF16)
            op("dve", "tensor_reduce", out=KBF, in_=K.rearrange("p c (n k) -> p c n k", n=8),
               axis=AX.X, op=ALU.add)
            op("dve", "tensor_copy", out=KBB, in_=KBF)
            for qs in range(8, 16):
                bq = qs // 2
                for h in range(8):
                    c, pbase = h // 2, (h % 2) * 64
                    op("pe", "matmul", out=PS(6)[:, h * 8:(h + 1) * 8],
                       lhsT=Q[pbase:pbase + 64, c, qs * 128:(qs + 1) * 128],
                       rhs=KBB[pbase:pbase + 64, c, :], start=True, stop=True)
                op("dve", "tensor_copy", out=GS, in_=PS(6)[:, 0:64].rearrange("p (h n) -> p h n", h=8))
                op("dve", "memset", outs=("ap",), ap=GS[:, :, bq:8], constant=NEG)
                for h in range(8):
                    op("dve", "max", out=M8[:, h, :], in_=GS[:, h, :])
                    op("dve", "tensor_scalar", out=SEL[:, qs, h, :], in0=GS[:, h, :],
                       scalar1=M8[:, h, 2:3], scalar2=None, op0=ALU.is_ge)
            steps = [(h, qt, kb) for h in range(8) for qt in range(NT) for kb in range(2 * qt + 2)]
            st_pts = {}

            def front(i):
                h, qt, kb = steps[i]
                par = i % 2
                c, pbase = h // 2, (h % 2) * 64
                wide = h >= 2
                s_lo = 0 if kb <= 2 * qt else 2
                qlo = qt * TT + s_lo * 128
                nq = TT - s_lo * 128
                pts = []
                for half in range(2):
                    kt = 2 * kb + half
                    if kt > qt * 4 + 3:
                        continue
                    psb = par * 2 + half
                    pt = PT[par * 2 + half]
                    op("pe", "matmul", out=PS(psb)[:, 0:nq],
                       lhsT=K[pbase:pbase + 64, c, kt * 128:(kt + 1) * 128],
                       rhs=Q[pbase:pbase + 64, c, qlo:qlo + nq], start=True, stop=True)
                    s_first = max(s_lo, kt - qt * 4)
                    if wide:
                        col = C_BIAS + h * 20 + (4 * qt - kt + 3)
                        o0 = (s_first - s_lo) * 128
                        op("act", "activation", out=pt[:, s_first * 128:TT],
                           in_=PS(psb)[:, o0:nq], func=AF.Exp, scale=0.125,
                           bias=CST[:, col:col + 1])
                    else:
                        for s_ in range(s_first, 4):
                            qs = qt * 4 + s_
                            col = C_BIAS + h * 20 + (qs - kt + 3)
                            o0 = (s_ - s_lo) * 128
                            op("act", "activation", out=pt[:, s_ * 128:(s_ + 1) * 128],
                               in_=PS(psb)[:, o0:o0 + 128], func=AF.Exp, scale=0.125,
                               bias=CST[:, col:col + 1])
                    if kt >= qt * 4:
                        s_ = kt - qt * 4
                        op("pool", "tensor_tensor", out=pt[:, s_ * 128:(s_ + 1) * 128],
                           in0=pt[:, s_ * 128:(s_ + 1) * 128], in1=TRIB[:], op=ALU.mult)
                    pts.append((kt, pt))
                st_pts[i] = pts

            def back(i):
                h, qt, kb = steps[i]
                par = i % 2
                s_lo = 0 if kb <= 2 * qt else 2
                pts = st_pts.pop(i)
                acc = ACC[(h * NT + qt) % 2]
                pso = PS(4 + par)
                for s_ in range(s_lo, 4):
                    qs = qt * 4 + s_
                    rel = [(kt, pt) for (kt, pt) in pts if kt <= qs]
                    for j, (kt, pt) in enumerate(rel):
                        op("pe", "matmul", out=pso[:, s_ * 65:(s_ + 1) * 65],
                           lhsT=pt[:, s_ * 128:(s_ + 1) * 128], rhs=V[:, kt, h, :],
                           start=(j == 0), stop=(j == len(rel) - 1))
                for s_ in range(s_lo, 4):
                    qs = qt * 4 + s_
                    bq = qs // 2
                    gated = (bq >= 4 and kb < bq)
                    wsc = SEL[:, qs, h, kb:kb + 1] if gated else 1.0
                    if kb == 0:
                        op("dve", "tensor_scalar", out=acc[:, s_, :], in0=pso[:, s_ * 65:(s_ + 1) * 65],
                           scalar1=wsc, scalar2=None, op0=ALU.mult)
                    else:
                        op("dve", "scalar_tensor_tensor", out=acc[:, s_, :],
                           in0=pso[:, s_ * 65:(s_ + 1) * 65], scalar=wsc, in1=acc[:, s_, :],
                           op0=ALU.mult, op1=ALU.add)
                    if kb == bq:
                        op("dve", "reciprocal", out=RCP[:, s_:s_ + 1], in_=acc[:, s_, 64:65])
                        op("dve", "tensor_scalar", out=OT[:, qs, h * 64:(h + 1) * 64],
                           in0=acc[:, s_, 0:64], scalar1=RCP[:, s_:s_ + 1], scalar2=None, op0=ALU.mult)

            front(0)
            for i in range(len(steps)):
                if i + 1 < len(steps):
                    front(i + 1)
                back(i)
            P.phase = "m0.otT"
            for qt in range(NT):
                for c in range(4):
                    pb = (qt * 4 + c) % 2
                    pst = PS(pb).bitcast(BF16)
                    for s in range(4):
                        op("pe", "transpose", out=pst[:, s * 128:(s + 1) * 128],
                           in_=OT[:, qt * 4 + s, c * 128:(c + 1) * 128], identity=IDB[:])
                    evac(HB[:, 4 + c, qt * TT:(qt + 1) * TT], pst[:, 0:512])
            P.phase = "m0"
            out_proj(ab_w_out, lambda kc, ts: HB[:, kc, ts], 32 * KB_, after_tile)

        def mixer1(after_tile):
            WBI = [vw(i * 4 * KB_, [KC, 256], BF16) for i in range(2)]
            WG = vw(8 * KB_, [2, 8, 128], BF16)
            TMP = vw(12 * KB_, [1024], F32)
            GG = [vw(16 * KB_ + i * 4 * KB_, [T], BF16) for i in range(2)]
            XRB = [vw(24 * KB_ + i * 4608, [T + 4], BF16) for i in range(2)]
            HT = T // 2
            XC = [vw(33 * KB_ + q * 18 * KB_, [HT], F32) for q in range(2)]
            XCB = [vw(37 * KB_ + q * 18 * KB_, [HT], BF16) for q in range(2)]
            RA = [vw(39 * KB_ + q * 18 * KB_, [HT], F32) for q in range(2)]
            IH = [vw(43 * KB_ + q * 18 * KB_, [HT], F32) for q in range(2)]
            TB = [vw(47 * KB_ + q * 18 * KB_, [HT], F32) for q in range(2)]
            Y = vw(69 * KB_, [KC, T], BF16)
            DG4 = vw(101 * KB_, [4, 128], BF16)
            P.phase = "m1.chunks"
            for i, wsrc in enumerate((c_w_r, c_w_i)):
                op("pool", "dma_start", dma_key=("wg", i), out=WG[:, i, :, :],
                   in_=wsrc.rearrange("h i j -> i h j"))
            for i in range(2):
                op("dve", "memset", outs=("ap",), ap=XRB[i][:, 0:3], constant=0.0)

            def stage_a(c, hf):
                W = WBI[c % 2]
                gg = GG[c % 2]
                xrb = XRB[c % 2]
                if hf == 0:
                    op("sp", "dma_start", dma_key=("wbi", c % 2), out=W, in_=cw_scr[c], after=rl_inst[c])
                hs = slice(hf * 1024, (hf + 1) * 1024)
                for t2 in range(2):
                    tt = hf * 2 + t2
                    ts = slice(tt * TT, (tt + 1) * TT)
                    for kc in range(KC):
                        op("pe", "matmul", out=PS(t2), lhsT=W[:, kc, 0:128], rhs=HB[:, kc, ts],
                           start=(kc == 0), stop=(kc == KC - 1))
                    yield
                g2 = PSR(0, 2)
                op("act", "activation", out=TMP, in_=g2, func=AF.Square, scale=GELU_C ** 0.5)
                yield
                for t2 in range(2):
                    tt = hf * 2 + t2
                    ts = slice(tt * TT, (tt + 1) * TT)
                    for kc in range(KC):
                        op("pe", "matmul", out=PS(2 + t2), lhsT=W[:, kc, 128:256], rhs=HB[:, kc, ts],
                           start=(kc == 0), stop=(kc == KC - 1))
                    yield
                op("dve", "scalar_tensor_tensor", out=TMP, in0=TMP, scalar=1.0, in1=g2,
                   op0=ALU.add, op1=ALU.mult)
                yield
                op("act", "activation", out=xrb[:, 3 + hf * 1024: 3 + (hf + 1) * 1024], in_=PSR(2, 2),
                   func=AF.Copy)
                yield
                op("act", "activation", out=TMP, in_=TMP, func=AF.Tanh, scale=GELU_S)
                yield
                op("dve", "scalar_tensor_tensor", out=gg[:, hs], in0=TMP, scalar=1.0, in1=g2,
                   op0=ALU.add, op1=ALU.mult)
                yield

            ucount = [0]

            def stage_b(c, hf):
                u = ucount[0]
                ucount[0] += 1
                q = u % 2
                gg = GG[c % 2]
                xrb = XRB[c % 2]
                hs = slice(hf * HT, (hf + 1) * HT)
                xc, xcb, ra, ih, tb = XC[q], XCB[q], RA[q], IH[q], TB[q]
                if hf == 0:
                    for j in range(4):
                        col = V_CCW + j * 8 + c
                        op("dve", "tensor_scalar", out=DG4[:, j, :], in0=IDB[:],
                           scalar1=VEC[:, col:col + 1], scalar2=None, op0=ALU.mult)
                    yield
                for t2 in range(2):
                    tt = hf * 2 + t2
                    for j in range(4):
                        op("pe", "matmul", out=PS(4 + t2), lhsT=DG4[:, j, :],
                           rhs=xrb[:, tt * TT + j: tt * TT + j + TT], start=(j == 0), stop=(j == 3))
                yield
                op("act", "activation", out=xcb, in_=PSR(4, 2), func=AF.Identity,
                   bias=VEC[:, V_CCB + c:V_CCB + c + 1])
                yield
                op("act", "activation", out=xc, in_=PSR(4, 2), func=AF.Identity,
                   bias=VEC[:, V_CCB + c:V_CCB + c + 1])
                yield
                for gi, dstb in ((0, ra), (1, ih)):
                    for t2 in range(2):
                        op("pe", "matmul", out=PS(6 + t2), lhsT=WG[:, gi, c, :],
                           rhs=xcb[:, t2 * TT:(t2 + 1) * TT], start=True, stop=True)
                    yield
                    op("act", "activation", out=dstb, in_=PSR(6, 2), func=AF.Tanh, scale=0.5,
                       bias=HV[:, gi * 8 + c: gi * 8 + c + 1])
                    yield
                op("act", "activation", out=ra, in_=ra, func=AF.Exp, scale=HV[:, 16 + c:17 + c],
                   bias=HV[:, 16 + c:17 + c])
                yield
                op("dve", "tensor_tensor", out=tb, in0=ra, in1=ra, op=ALU.mult)
                yield
                op("dve", "tensor_scalar", out=tb, in0=tb, scalar1=1.0, scalar2=-1.0, op0=ALU.min, op1=ALU.mult)
                yield
                op("act", "activation", out=tb, in_=tb, func=AF.Sqrt, bias=1.0)
                yield
                op("dve", "scalar_tensor_tensor", out=tb, in0=ih, scalar=1.0, in1=tb, op0=ALU.add, op1=ALU.mult)
                yield
                op("dve", "scalar_tensor_tensor", out=tb, in0=tb, scalar=0.25, in1=xc, op0=ALU.mult, op1=ALU.mult)
                yield
                init = 0.0 if hf == 0 else IH[1 - q][:, HT - 1:HT]
                op("dve", "tensor_tensor_scan", out=ih, data0=ra, data1=tb, initial=init,
                   op0=ALU.mult, op1=ALU.add)
                yield
                op("dve", "tensor_tensor", out=Y[:, c, hs], in0=ih, in1=gg[:, hs], op=ALU.mult)
                yield

            def zip_run(*gens):
                gens = [g for g in gens if g is not None]
                while gens:
                    for g in list(gens):
                        try:
                            next(g)
                        except StopIteration:
                            gens.remove(g)

            zip_run(stage_a(0, 0))
            zip_run(stage_a(0, 1))
            for c in range(KC):
                if ZIP_M1:
                    for hf in range(2):
                        zip_run(stage_b(c, hf), stage_a(c + 1, hf) if c + 1 < KC else None)
                else:
                    if c + 1 < KC:
                        zip_run(stage_a(c + 1, 0))
                        zip_run(stage_a(c + 1, 1))
                    zip_run(stage_b(c, 0))
                    zip_run(stage_b(c, 1))
            P.phase = "m1"
            out_proj(c_w_out, lambda kc, ts: Y[:, kc, ts], 33 * KB_, after_tile)

        def nothing(tt):
            pass

        for s in range(n_seq):
            nt0 = (lambda tt: norm_tile(V_MIX + 0, hb_dst, tt))
            nt1 = (lambda tt: norm_tile(V_MLP + 0, hb_dst, tt))
            nt2 = (lambda tt: norm_tile(V_MIX + 8, hb_dst, tt))
            nt3 = (lambda tt: norm_tile(V_MLP + 8, hb_dst, tt))
            if s == 0 or stop_stage < 5:
                load_x(s, nt0 if stop_stage >= 1 else nothing)
            if stop_stage >= 1:
                mixer0(nt1 if stop_stage >= 2 else nothing)
            if stop_stage >= 2:
                mlp(0, nt2 if stop_stage >= 3 else nothing)
            if stop_stage >= 3:
                mixer1(nt3 if stop_stage >= 4 else nothing)
            if stop_stage >= 4:
                def tail_cb(tt, s=s):
                    store_tile(s, tt, True, deferred=True)
                    if s + 1 < n_seq:
                        preload_tile(s + 1, tt)
                mlp(1, tail_cb if stop_stage >= 5 else nothing)
            if stop_stage < 5:
                for tt in range(NT):
                    store_tile(s, tt, False)
        P.emit(final_wait_keys=[("os", 0)])
        info = dict(n_inst=P.n, per_eng={e: len(P.insts[e]) for e in ENGS}, sig=P.sig_counts,
                    phases={e: [i.phase for i in P.insts[e]] for e in ENGS})
    return nc, info


def _feat(v):
    v = np.asarray(v, dtype=np.float32)
    return np.ascontiguousarray(v.reshape(-1, 128).T)


def make_vecs(inp):
    cols = np.zeros((128, NV), np.float32)
    for l in range(2):
        cols[:, V_MIX + l * 8:V_MIX + l * 8 + 8] = _feat(inp["mix_norm"][l])
        cols[:, V_MLP + l * 8:V_MLP + l * 8 + 8] = _feat(inp["mlp_norm"][l])
    cols[:, V_FIN:V_FIN + 8] = _feat(inp["final_norm"])
    cols[:, V_LNG:V_LNG + 4] = _feat(inp["ab_ln_g"][0])
    cols[:, V_LNB:V_LNB + 4] = _feat(inp["ab_ln_b"][0])
    for j in range(31):
        cols[:, V_CW + j * 4:V_CW + j * 4 + 4] = _feat(inp["ab_conv_w"][0][j])
    for j in range(4):
        cols[:, V_CCW + j * 8:V_CCW + j * 8 + 8] = _feat(inp["c_conv_w"][0][j])
    cols[:, V_CCB:V_CCB + 8] = _feat(inp["c_conv_b"][0])
    cols[:, V_BR:V_BR + 8] = _feat(np.asarray(inp["c_b_r"][0]).reshape(-1))
    cols[:, V_BI:V_BI + 8] = _feat(np.asarray(inp["c_b_i"][0]).reshape(-1))
    cols[:, V_LAM:V_LAM + 8] = _feat(inp["c_lambda"][0])
    return cols


def make_consts():
    c = np.zeros((128, NCST), np.float32)
    c[:, C_ID:C_ID + 128] = np.eye(128, dtype=np.float32)
    p = np.arange(128)
    for h in range(8):
        slope = 2.0 ** (-(h + 1))
        for i in range(20):
            c[:, C_BIAS + h * 20 + i] = slope * (p - 128.0 * (i - 3))
    c[:, C_M05] = -0.5
    c[:, C_P05] = 0.5
    return c


def make_tri():
    p = np.arange(128)
    return (p[:, None] <= p[None, :]).astype(np.float32)


_CACHE = {}


def run(inputs, n_cores=N_CORES, n_seq=SEQ_PER_CORE, stop_stage=5, seq_ids=None, trace=False):
    key = (n_seq, stop_stage)
    if key not in _CACHE:
        _CACHE[key] = build(n_seq, stop_stage)
    nc, info = _CACHE[key]
    f = lambda a: np.ascontiguousarray(np.asarray(a, dtype=np.float32))
    shared = {
        "vecs": make_vecs(inputs), "consts": make_consts(), "tri": make_tri(),
        "w_up": f(inputs["w_up"]), "w_down": f(inputs["w_down"]),
        "ab_w_in": f(inputs["ab_w_in"][0]), "ab_w_out": f(inputs["ab_w_out"][0]),
        "c_w_in": f(inputs["c_w_in"][0]), "c_w_out": f(inputs["c_w_out"][0]),
        "c_w_r": f(inputs["c_w_r"][0]), "c_w_i": f(inputs["c_w_i"][0]),
    }
    x = inputs["x"]
    in_maps = []
    for ci in range(n_cores):
        if seq_ids is None:
            xs = x[ci * n_seq:(ci + 1) * n_seq]
        else:
            xs = x[seq_ids[ci]]
        m = dict(shared)
        m["x"] = f(xs)
        in_maps.append(m)
    res = run_bass_kernel_spmd(nc, in_maps, core_ids=list(range(n_cores)), trace=trace)
    outs = [r["y"] for r in res.results]
    return np.concatenate(outs, axis=0), res, info


def kernel(**inputs):
    y, _, _ = run(inputs)
    return y.astype(np.float32)
```

```python
import contextlib
import numpy as np
import concourse.bass as bass
import concourse.mybir as mybir
from concourse.bass_utils import run_bass_kernel_spmd

F32 = mybir.dt.float32
BF16 = mybir.dt.bfloat16
ALU = mybir.AluOpType
AF = mybir.ActivationFunctionType
AX = mybir.AxisListType

ENGS = ["pe", "act", "dve", "pool", "sp"]
_DT_SIZE = {"float32": 4, "bfloat16": 2, "float16": 2, "int32": 4, "uint32": 4,
            "uint8": 1, "int8": 1, "uint16": 2, "int16": 2, "float32r": 4}


def _dsize(dt):
    return _DT_SIZE[str(dt).split(".")[-1]]


def _is_ap(v):
    return hasattr(v, "tensor") and hasattr(v, "ap") and hasattr(v, "offset")


def _footprint(ap):
    tn = type(ap.tensor).__name__
    if tn.startswith("DRam"):
        return None
    dims = ap.ap
    es = _dsize(ap.dtype)
    pstride = dims[0][0]
    off = int(ap.offset)
    if pstride > 0:
        p0 = off // pstride
        foff = off % pstride
    else:
        p0 = 0
        foff = off
    p1 = p0 + dims[0][1]
    free = [(abs(s), n) for (s, n) in dims[1:] if s != 0 and n > 1]
    if not free:
        if tn.startswith("PSum"):
            b0 = (foff * es // 2048) * 2048
            return (ap.tensor.name, p0, p1, [(b0, b0 + 2048)])
        return (ap.tensor.name, p0, p1, [(foff * es, (foff + 1) * es)])
    free.sort()
    s0, n0 = free[0]
    if s0 == 1:
        run = n0
        rest = free[1:]
        while rest and rest[0][0] == run:
            run *= rest[0][1]
            rest = rest[1:]
    else:
        run = 1
        rest = free
    cnt = 1
    for s, n in rest:
        cnt *= n
    if cnt > 64:
        span = run + sum(s * (n - 1) for s, n in rest)
        lo, hi = foff * es, (foff + span) * es
        if tn.startswith("PSum"):
            lo, hi = (lo // 2048) * 2048, ((hi + 2047) // 2048) * 2048
        return (ap.tensor.name, p0, p1, [(lo, hi)])
    starts = [foff]
    for s, n in rest:
        starts = [b + i * s for b in starts for i in range(n)]
    ivs = sorted((b * es, (b + run) * es) for b in starts)
    if tn.startswith("PSum"):
        ivs = sorted(set(((lo // 2048) * 2048, ((hi + 2047) // 2048) * 2048) for lo, hi in ivs))
    out = [ivs[0]]
    for lo, hi in ivs[1:]:
        if lo <= out[-1][1]:
            out[-1] = (out[-1][0], max(hi, out[-1][1]))
        else:
            out.append((lo, hi))
    return (ap.tensor.name, p0, p1, out)


def _ov(a, b):
    if a[0] >= b[1] or b[0] >= a[1]:
        return False
    for lo, hi in a[2]:
        for lo2, hi2 in b[2]:
            if lo < hi2 and lo2 < hi:
                return True
    return False


def _covers(w, o):
    if not (w[0] <= o[0] and o[1] <= w[1]):
        return False
    for lo, hi in o[2]:
        ok = False
        for lo2, hi2 in w[2]:
            if lo2 <= lo and hi <= hi2:
                ok = True
                break
        if not ok:
            return False
    return True


class Inst:
    __slots__ = ("eng", "fn", "deps", "is_dma", "key", "kord", "ordn", "signal", "sigval", "phase")


class Prog:
    def __init__(self, nc):
        self.nc = nc
        self.insts = {e: [] for e in ENGS}
        self.acc = {}
        self.key_count = {}
        self.n = 0
        self.phase = ""

    def op(self, eng, method, outs=("out", "accum_out"), extra_reads=(), dma_key=None, after=(), **kw):
        reads, writes = [], []
        for k, v in kw.items():
            if _is_ap(v):
                fp = _footprint(v)
                if fp is None:
                    continue
                (writes if k in outs else reads).append(fp)
        for v in extra_reads:
            fp = _footprint(v)
            if fp is not None:
                reads.append(fp)
        ins = Inst()
        ins.eng = eng
        ins.phase = self.phase
        ins.fn = (method, kw)
        ins.is_dma = method == "dma_start"
        ins.signal = False
        ins.sigval = 0
        ins.ordn = len(self.insts[eng])
        if ins.is_dma:
            assert dma_key is not None
            c = self.key_count.get(dma_key, 0) + 1
            self.key_count[dma_key] = c
            ins.key = dma_key
            ins.kord = c
            token = ("D", dma_key, c)
        else:
            ins.key = None
            ins.kord = 0
            token = ("E", eng, ins.ordn)
        deps = {}

        def add_dep(tok):
            if tok[0] == "E":
                if tok[1] == "pe" and eng == "pe" and not ins.is_dma:
                    return
                k = ("E", tok[1])
            else:
                k = ("D", tok[1])
            if deps.get(k, -1) < tok[2]:
                deps[k] = tok[2]

        for a_ in after:
            add_dep(("D", a_.key, a_.kord) if a_.is_dma else ("E", a_.eng, a_.ordn))
        for (name, p0, p1, ivs) in reads:
            fpr = (p0, p1, ivs)
            for rec in self.acc.get(name, ()):
                if rec[0] and _ov(fpr, rec[2]):
                    add_dep(rec[1])
        for (name, p0, p1, ivs) in writes:
            fpr = (p0, p1, ivs)
            for rec in self.acc.get(name, ()):
                if _ov(fpr, rec[2]):
                    add_dep(rec[1])
        ins.deps = deps
        for (name, p0, p1, ivs) in writes:
            fpr = (p0, p1, ivs)
            lst = self.acc.setdefault(name, [])
            lst[:] = [r for r in lst if not _covers(fpr, r[2])]
            lst.append((True, token, fpr))
        for (name, p0, p1, ivs) in reads:
            fpr = (p0, p1, ivs)
            lst = self.acc.setdefault(name, [])
            if token[0] == "E":
                lst[:] = [r for r in lst if not ((not r[0]) and r[1][0] == "E" and r[1][1] == eng
                                                 and r[2] == fpr)]
            lst.append((False, token, fpr))
        self.insts[eng].append(ins)
        self.n += 1
        return ins

    def emit(self, final_wait_keys=()):
        nc = self.nc
        for e in ENGS:
            for ins in self.insts[e]:
                for k, v in ins.deps.items():
                    if k[0] == "E":
                        self.insts[k[1]][v].signal = True
        for e in ENGS:
            c = 0
            for ins in self.insts[e]:
                if ins.signal and not ins.is_dma:
                    c += 1
                ins.sigval = c
        self.sig_counts = {e: (self.insts[e][-1].sigval if self.insts[e] else 0) for e in ENGS}
        with contextlib.ExitStack() as st:
            esem = {e: st.enter_context(nc.semaphore("s_" + e)) for e in ENGS}
            ksem = {k: st.enter_context(nc.semaphore("k_%s" % str(k))) for k in self.key_count}
            block = st.enter_context(nc.Block())
            prog = self

            def run(e, engobj):
                known = {}
                for ins in prog.insts[e]:
                    for k, v in ins.deps.items():
                        if k[0] == "E":
                            sem = esem[k[1]]
                            val = prog.insts[k[1]][v].sigval
                        else:
                            sem = ksem[k[1]]
                            val = 16 * v
                        if known.get(k, 0) < val:
                            engobj.wait_ge(sem, val)
                            known[k] = val
                    method, kw = ins.fn
                    bi = getattr(engobj, method)(**kw)
                    if ins.is_dma:
                        bi.then_inc(ksem[ins.key], 16)
                    elif ins.signal:
                        bi.then_inc(esem[e], 1)
                if e == "sp":
                    for k in final_wait_keys:
                        engobj.wait_ge(ksem[k], 16 * prog.key_count[k])

            @block.tensor
            def _(eng):
                run("pe", eng)

            @block.scalar
            def _(eng):
                run("act", eng)

            @block.vector
            def _(eng):
                run("dve", eng)

            @block.gpsimd
            def _(eng):
                run("pool", eng)

            @block.sync
            def _(eng):
                run("sp", eng)


T = 2048
D = 1024
KC = 8
TT = 512
NT = 4
EPS = 1e-6
NEG = -1.0e30
N_CORES = 8
SEQ_PER_CORE = 2

V_MIX = 0
V_MLP = 16
V_FIN = 32
V_LNG = 40
V_LNB = 44
V_CW = 48
V_CCW = 172
V_CCB = 204
V_BR = 212
V_BI = 220
V_LAM = 228
NV = 236
C_ID = 0
C_BIAS = 128
C_M05 = 288
C_P05 = 289
NCST = 290

ARENA_BYTES = 107008
KB_ = 1024
GELU_C = 0.044715
GELU_S = 0.7978845608028654
USE_POOL_POW = False
ZIP_M1 = True


def build(n_seq=SEQ_PER_CORE, stop_stage=5):
    nc = bass.Bass("TRN2", target_bir_lowering=False)
    dr = {}

    def din(name, shape):
        dr[name] = nc.dram_tensor(name, list(shape), F32, kind="ExternalInput").ap()
        return dr[name]

    x_in = din("x", [n_seq, T, D])
    vecs_in = din("vecs", [128, NV])
    cst_in = din("consts", [128, NCST])
    tri_in = din("tri", [128, 128])
    w_up = din("w_up", [2, D, 4 * D])
    w_down = din("w_down", [2, 4 * D, D])
    ab_w_in = din("ab_w_in", [D, 2560])
    ab_w_out = din("ab_w_out", [D, D])
    c_w_in = din("c_w_in", [D, 2 * D])
    c_w_out = din("c_w_out", [D, D])
    c_w_r = din("c_w_r", [8, 128, 128])
    c_w_i = din("c_w_i", [8, 128, 128])
    y_out = nc.dram_tensor("y", [n_seq, T, D], F32, kind="ExternalOutput").ap()
    cw_scr = nc.dram_tensor("cw_scr", [8, 128, 8, 256], BF16).ap()

    with contextlib.ExitStack() as st:
        def sb(name, shape, dt):
            return st.enter_context(nc.sbuf_tensor(name, shape, dt))

        X = sb("X", [128, KC, T], F32)
        HB = sb("HB", [128, KC, T], BF16)
        VEC = sb("VEC", [128, NV], F32)
        CST = sb("CST", [128, NCST], F32)
        IDB = sb("IDB", [128, 128], BF16)
        TRIB = sb("TRIB", [128, 128], BF16)
        ONE1024 = sb("ONE1024", [128, 128], BF16)
        ONE512 = sb("ONE512", [128, 128], BF16)
        HV = sb("HV", [128, 32], F32)
        NSQ = sb("NSQ", [128, 2, TT], BF16)
        NRS = sb("NRS", [128, TT], F32)
        AR = sb("AR", [128, ARENA_BYTES // 2], BF16)
        PSA = st.enter_context(nc.psum_tensor("PSA", [128, 8, 512], F32))

        P = Prog(nc)
        op = P.op

        def PS(i):
            return PSA[:, i, :]

        def PSR(i, n):
            return PSA[:, i:i + n, :].rearrange("p a b -> p (a b)")

        def vw(off, shape, dt):
            es = 4 if dt == F32 else 2
            n = int(np.prod(shape))
            assert off % 4 == 0 and off + n * es <= ARENA_BYTES, (off, shape)
            ap = AR[:, off // 2: off // 2 + n * es // 2]
            if dt == F32:
                ap = ap.bitcast(F32)
            if len(shape) == 2:
                ap = ap.rearrange("p (a b) -> p a b", a=shape[0])
            elif len(shape) == 3:
                ap = ap.rearrange("p (a b c) -> p a b c", a=shape[0], b=shape[1])
            elif len(shape) == 4:
                ap = ap.rearrange("p (a b c d) -> p a b c d", a=shape[0], b=shape[1], c=shape[2])
            return ap

        cnt = [0]
        rl_inst = {}
        from collections import deque
        side = deque()

        def relayout_cw():
            for c in range(KC):
                lst = []
                for part in range(2):
                    lst.append(op("pool", "dma_start", dma_key=("rl", c, part),
                                  out=cw_scr[c][:, :, part * 128:(part + 1) * 128],
                                  in_=c_w_in[:, part * D + c * 128: part * D + (c + 1) * 128]
                                  .rearrange("(kc p) m -> p kc m", p=128)))
                rl_inst[c] = lst

        def evac(out, in_):
            cnt[0] += 1
            if cnt[0] % 2:
                op("act", "activation", out=out, in_=in_, func=AF.Copy)
            else:
                op("dve", "tensor_copy", out=out, in_=in_)

        M05 = CST[:, C_M05:C_M05 + 1]
        P05 = CST[:, C_P05:C_P05 + 1]
        IDF = CST[:, C_ID:C_ID + 128]

        P.phase = "setup"
        op("sp", "dma_start", dma_key="vec", out=VEC[:], in_=vecs_in)
        op("sp", "dma_start", dma_key="cst", out=CST[:], in_=cst_in)
        op("pool", "dma_start", dma_key="tri", out=TRIB[:], in_=tri_in)
        op("dve", "tensor_copy", out=IDB[:], in_=CST[:, C_ID:C_ID + 128])
        op("dve", "memset", outs=("ap",), ap=ONE1024[:], constant=1.0 / 1024)
        op("dve", "memset", outs=("ap",), ap=ONE512[:], constant=1.0 / 512)
        op("act", "activation", out=HV[:, 24:32], in_=VEC[:, V_LAM:V_LAM + 8], func=AF.Exp, scale=-1.0)
        op("act", "activation", out=HV[:, 24:32], in_=HV[:, 24:32], func=AF.Ln, bias=1.0)
        op("dve", "tensor_scalar", out=HV[:, 16:24], in0=HV[:, 24:32], scalar1=-4.0, scalar2=None, op0=ALU.mult)
        op("dve", "tensor_scalar", out=HV[:, 0:8], in0=VEC[:, V_BR:V_BR + 8], scalar1=0.5, scalar2=None, op0=ALU.mult)
        op("dve", "tensor_scalar", out=HV[:, 8:16], in0=VEC[:, V_BI:V_BI + 8], scalar1=0.5, scalar2=None, op0=ALU.mult)

        def load_w(dst, src, key, nk=KC):
            for kc in range(nk):
                op("pool", "dma_start", dma_key=(key, kc), out=dst[:, kc, :],
                   in_=src[kc * 128:(kc + 1) * 128, :])

        def norm_tile(gcol, dst, tt):
            ph = P.phase
            P.phase = "norm"
            ts = slice(tt * TT, (tt + 1) * TT)
            for c in range(KC):
                op("act", "activation", out=NSQ[:, c % 2, :], in_=X[:, c, ts], func=AF.Square)
                op("pe", "matmul", out=PS(7), lhsT=ONE1024[:], rhs=NSQ[:, c % 2, :],
                   start=(c == 0), stop=(c == KC - 1))
            op("act", "activation", out=NRS[:], in_=PS(7), func=AF.Sqrt, bias=EPS)
            op("dve", "reciprocal", out=NRS[:], in_=NRS[:])
            for c in range(KC):
                op("dve", "scalar_tensor_tensor", out=dst(c, tt), in0=X[:, c, ts],
                   scalar=VEC[:, gcol + c:gcol + c + 1], in1=NRS[:], op0=ALU.mult, op1=ALU.mult)
            P.phase = ph

        def hb_dst(c, tt):
            return HB[:, c, tt * TT:(tt + 1) * TT]

        def mlp(l, after_tile, swap=False):
            WS = [vw(i * 16 * KB_, [KC, 1024], BF16) for i in range(4)]
            if swap:
                WS = [WS[2], WS[3], WS[0], WS[1]]
            Hb = [vw(64 * KB_ + i * 8 * KB_, [KC, TT], BF16) for i in range(2)]
            RL = [vw(80 * KB_ + i * 2 * KB_, [TT], F32) for i in range(2)]
            k = 0
            for g in range(4):
                P.phase = "mlp%d.g%d" % (l, g)
                WU = WS[(2 * g) % 4]
                WD = WS[(2 * g + 1) % 4]
                sw = 2 if swap else 0
                load_w(WU, w_up[l][:, g * 1024:(g + 1) * 1024], "ws%d" % ((2 * g + sw) % 4))
                load_w(WD, w_down[l][g * 1024:(g + 1) * 1024, :], "ws%d" % ((2 * g + 1 + sw) % 4))
                for tt in range(NT):
                    ts = slice(tt * TT, (tt + 1) * TT)
                    H = Hb[tt % 2]
                    for hc in range(KC):
                        pb = k % 3
                        k += 1
                        for kc in range(KC):
                            op("pe", "matmul", out=PS(pb), lhsT=WU[:, kc, hc * 128:(hc + 1) * 128],
                               rhs=HB[:, kc, ts], start=(kc == 0), stop=(kc == KC - 1))
                        op("act", "activation", out=RL[hc % 2], in_=PS(pb), func=AF.Relu)
                        op("act", "activation", out=H[:, hc, :], in_=RL[hc % 2], func=AF.Square)
                        if side:
                            side.popleft()()
                    for m in range(KC):
                        pb = 3 + (k % 3)
                        k += 1
                        for hc in range(KC):
                            op("pe", "matmul", out=PS(pb), lhsT=WD[:, hc, m * 128:(m + 1) * 128],
                               rhs=H[:, hc, :], start=(hc == 0), stop=(hc == KC - 1))
                        op("dve", "tensor_tensor", out=X[:, m, ts], in0=PS(pb), in1=X[:, m, ts], op=ALU.add)
                        if side:
                            side.popleft()()
                    if g == 3:
                        after_tile(tt)
            while side:
                side.popleft()()

        def out_proj(w_src, YC, wb_off, after_tile):
            offs = wb_off if isinstance(wb_off, (list, tuple)) else [wb_off, wb_off + 8 * KB_]
            WB = [vw(offs[i], [KC, 512], BF16) for i in range(2)]
            k = 0
            P.phase = P.phase.split(".")[0] + ".oproj"
            for hh in range(2):
                load_w(WB[hh], w_src[:, hh * 512:(hh + 1) * 512], "wo%d" % hh)
            for hh in range(2):
                for tt in range(NT):
                    ts = slice(tt * TT, (tt + 1) * TT)
                    for m in range(4):
                        pb = k % 4
                        k += 1
                        for kc in range(KC):
                            op("pe", "matmul", out=PS(pb), lhsT=WB[hh][:, kc, m * 128:(m + 1) * 128],
                               rhs=YC(kc, ts), start=(kc == 0), stop=(kc == KC - 1))
                        mm = hh * 4 + m
                        op("dve", "tensor_tensor", out=X[:, mm, ts], in0=PS(pb), in1=X[:, mm, ts], op=ALU.add)
                    if hh == 1:
                        after_tile(tt)

        def load_x(s, after_tile):
            P.phase = "load"
            XS = [vw(i * 4 * KB_, [D], F32) for i in range(3)]
            for t16 in range(16):
                xs = XS[t16 % 3]
                op("sp", "dma_start", dma_key=("xs", t16 % 3), out=xs,
                   in_=x_in[s, t16 * 128:(t16 + 1) * 128, :])
                for half in range(2):
                    pb = (t16 * 2 + half) % 4
                    for cc in range(4):
                        c = half * 4 + cc
                        op("pe", "transpose", out=PS(pb)[:, cc * 128:(cc + 1) * 128],
                           in_=xs[:, c * 128:(c + 1) * 128], identity=IDF)
                    evac(X[:, half * 4:half * 4 + 4, t16 * 128:(t16 + 1) * 128],
                         PS(pb).rearrange("p (a b) -> p a b", a=4))
                if t16 % 4 == 3:
                    after_tile(t16 // 4)

        def store_tile(s, tt, do_norm, deferred=False):
            while side:
                side.popleft()()
            ph = P.phase
            P.phase = "store"
            XN = vw(84 * KB_, [KC, TT], F32)
            OS = vw(100 * KB_, [D], F32)
            if do_norm:
                norm_tile(V_FIN, lambda c, t_: XN[:, c, :], tt)
            P.phase = ph

            def job(j, half):
                def run_():
                    ph2 = P.phase
                    P.phase = "store"
                    t16 = tt * 4 + j
                    for cc in range(4):
                        c = half * 4 + cc
                        src = XN[:, c, j * 128:(j + 1) * 128] if do_norm else \
                            X[:, c, t16 * 128:(t16 + 1) * 128]
                        op("pe", "transpose", out=PS(6)[:, cc * 128:(cc + 1) * 128],
                           in_=src, identity=IDF)
                    evac(OS[:, half * 512:(half + 1) * 512], PS(6))
                    if half == 1:
                        op("sp", "dma_start", dma_key=("os", 0),
                           out=y_out[s, t16 * 128:(t16 + 1) * 128, :], in_=OS)
                    P.phase = ph2
                return run_
            for j in range(4):
                for half in range(2):
                    if deferred:
                        side.append(job(j, half))
                    else:
                        job(j, half)()

        def mixer0(after_tile):
            S0 = 0
            ZW = 2080
            Zo = 16128
            Z = vw(Zo, [4, ZW], BF16)
            WB = [vw(32 * KB_ + i * 8 * KB_, [KC, 512], BF16) for i in range(2)]
            DG = vw(48 * KB_, [31, 128], BF16)
            Qo = 56 * KB_
            Q = vw(Qo, [4, T], BF16)
            K = vw(Qo + 16 * KB_, [4, T], BF16)
            V = vw(Qo + 32 * KB_, [16, 8, 65], BF16)
            P.phase = "m0.win"
            op("dve", "memset", outs=("ap",), ap=Z[:, :, 0:30], constant=0.0)
            op("dve", "memset", outs=("ap",), ap=V[:, :, :, 64:65], constant=1.0)
            groups = [("gate", 512), ("val", 0), ("q", 1024), ("k", 1536), ("v", 2048)]
            k = 0
            for gi, (gname, col0) in enumerate(groups):
                W = WB[gi % 2]
                load_w(W, ab_w_in[:, col0:col0 + 512], "wb%d" % (gi % 2))
                if gname == "v":
                    for t16 in range(16):
                        pb = k % 4
                        k += 1
                        for kc in range(KC):
                            op("pe", "matmul", out=PS(pb), lhsT=HB[:, kc, t16 * 128:(t16 + 1) * 128],
                               rhs=W[:, kc, :], start=(kc == 0), stop=(kc == KC - 1))
                        evac(V[:, t16, :, 0:64], PS(pb).rearrange("p (h d) -> p h d", h=8))
                    continue
                for tt in range(NT):
                    ts = slice(tt * TT, (tt + 1) * TT)
                    zs = slice(30 + tt * TT, 30 + (tt + 1) * TT)
                    for m in range(4):
                        pb = k % 4
                        k += 1
                        for kc in range(KC):
                            op("pe", "matmul", out=PS(pb), lhsT=W[:, kc, m * 128:(m + 1) * 128],
                               rhs=HB[:, kc, ts], start=(kc == 0), stop=(kc == KC - 1))
                        if gname == "gate":
                            op("act", "activation", out=Z[:, m, zs], in_=PS(pb), func=AF.Sigmoid)
                        elif gname == "val":
                            op("dve", "tensor_tensor", out=Z[:, m, zs], in0=PS(pb), in1=Z[:, m, zs],
                               op=ALU.mult)
                        elif gname == "q":
                            evac(Q[:, m, ts], PS(pb))
                        else:
                            evac(K[:, m, ts], PS(pb))
            P.phase = "m0.conv"
            if not rl_inst:
                relayout_cw()
            for c in range(4):
                for j in range(31):
                    col = V_CW + j * 4 + c
                    op("dve", "tensor_scalar", out=DG[:, j, :], in0=IDB[:],
                       scalar1=VEC[:, col:col + 1], scalar2=None, op0=ALU.mult)
                for tt in (3, 2, 1, 0):
                    pb = k % 4
                    k += 1
                    for j in range(31):
                        op("pe", "matmul", out=PS(pb), lhsT=DG[:, j, :],
                           rhs=Z[:, c, tt * TT + j: tt * TT + j + TT], start=(j == 0), stop=(j == 30))
                    op("act", "activation", out=Z[:, c, 30 + tt * TT: 30 + (tt + 1) * TT], in_=PS(pb),
                       func=AF.Copy)
            P.phase = "m0.ln"
            SQ = vw(S0, [4, TT], BF16)
            MEAN = vw(S0 + 4 * KB_, [TT], F32)
            VAR = vw(S0 + 6 * KB_, [TT], F32)
            TM = [vw(S0 + 8 * KB_ + i * 2 * KB_, [TT], F32) for i in range(2)]
            for tt in range(NT):
                zs = slice(30 + tt * TT, 30 + (tt + 1) * TT)
                ts = slice(tt * TT, (tt + 1) * TT)
                for c in range(4):
                    op("act", "activation", out=SQ[:, c, :], in_=Z[:, c, zs], func=AF.Square)
                for c in range(4):
                    op("pe", "matmul", out=PS(4), lhsT=ONE512[:], rhs=Z[:, c, zs], start=(c == 0), stop=(c == 3))
                for c in range(4):
                    op("pe", "matmul", out=PS(5), lhsT=ONE512[:], rhs=SQ[:, c, :], start=(c == 0), stop=(c == 3))
                op("act", "activation", out=MEAN, in_=PS(4), func=AF.Copy)
                op("dve", "tensor_tensor", out=VAR, in0=MEAN, in1=MEAN, op=ALU.mult)
                op("dve", "tensor_tensor", out=VAR, in0=PS(5), in1=VAR, op=ALU.subtract)
                op("act", "activation", out=VAR, in_=VAR, func=AF.Sqrt, bias=EPS)
                op("dve", "reciprocal", out=VAR, in_=VAR)
                for c in range(4):
                    tm = TM[c % 2]
                    op("dve", "tensor_tensor", out=tm, in0=Z[:, c, zs], in1=MEAN, op=ALU.subtract)
                    op("dve", "tensor_tensor", out=tm, in0=tm, in1=VAR, op=ALU.mult)
                    op("act", "activation", out=HB[:, c, ts], in_=tm, func=AF.Silu,
                       scale=VEC[:, V_LNG + c:V_LNG + c + 1], bias=VEC[:, V_LNB + c:V_LNB + c + 1])
            P.phase = "m0.attn"
            A0 = S0
            KBF = vw(A0, [4, 8], F32)
            KBB = vw(A0 + 128, [4, 8], BF16)
            GS = vw(A0 + 256, [8, 8], F32)
            M8 = vw(A0 + 512, [8, 8], F32)
            RCP = vw(A0 + 768, [4], F32)
            SEL = vw(A0 + 1 * KB_, [16, 8, 8], F32)
            PT = [vw(A0 + 5 * KB_ + i * KB_, [TT], BF16) for i in range(4)]
            ACC = [vw(A0 + 9 * KB_ + i * 1536, [4, 65], F32) for i in range(2)]
            OT = vw(Zo, [16, 512], BF16)
            op("dve", "tensor_reduce", out=KBF, in_=K.rearrange("p c (n k) -> p c n k", n=8),
               axis=AX.X, op=ALU.add)
            op("dve", "tensor_copy", out=KBB, in_=KBF)
            for qs in range(8, 16):
                bq = qs // 2
                for h in range(8):
                    c, pbase = h // 2, (h % 2) * 64
                    op("pe", "matmul", out=PS(6)[:, h * 8:(h + 1) * 8],
                       lhsT=Q[pbase:pbase + 64, c, qs * 128:(qs + 1) * 128],
                       rhs=KBB[pbase:pbase + 64, c, :], start=True, stop=True)
                op("dve", "tensor_copy", out=GS, in_=PS(6)[:, 0:64].rearrange("p (h n) -> p h n", h=8))
                op("dve", "memset", outs=("ap",), ap=GS[:, :, bq:8], constant=NEG)
                for h in range(8):
                    op("dve", "max", out=M8[:, h, :], in_=GS[:, h, :])
                    op("dve", "tensor_scalar", out=SEL[:, qs, h, :], in0=GS[:, h, :],
                       scalar1=M8[:, h, 2:3], scalar2=None, op0=ALU.is_ge)
            steps = [(h, qt, kb) for h in range(8) for qt in range(NT) for kb in range(2 * qt + 2)]
            st_pts = {}

            def front(i):
                h, qt, kb = steps[i]
                par = i % 2
                c, pbase = h // 2, (h % 2) * 64
                wide = h >= 2
                s_lo = 0 if kb <= 2 * qt else 2
                qlo = qt * TT + s_lo * 128
                nq = TT - s_lo * 128
                pts = []
                for half in range(2):
                    kt = 2 * kb + half
                    if kt > qt * 4 + 3:
                        continue
                    psb = par * 2 + half
                    pt = PT[par * 2 + half]
                    op("pe", "matmul", out=PS(psb)[:, 0:nq],
                       lhsT=K[pbase:pbase + 64, c, kt * 128:(kt + 1) * 128],
                       rhs=Q[pbase:pbase + 64, c, qlo:qlo + nq], start=True, stop=True)
                    s_first = max(s_lo, kt - qt * 4)
                    if wide:
                        col = C_BIAS + h * 20 + (4 * qt - kt + 3)
                        o0 = (s_first - s_lo) * 128
                        op("act", "activation", out=pt[:, s_first * 128:TT],
                           in_=PS(psb)[:, o0:nq], func=AF.Exp, scale=0.125,
                           bias=CST[:, col:col + 1])
                    else:
                        for s_ in range(s_first, 4):
                            qs = qt * 4 + s_
                            col = C_BIAS + h * 20 + (qs - kt + 3)
                            o0 = (s_ - s_lo) * 128
                            op("act", "activation", out=pt[:, s_ * 128:(s_ + 1) * 128],
                               in_=PS(psb)[:, o0:o0 + 128], func=AF.Exp, scale=0.125,
                               bias=CST[:, col:col + 1])
                    if kt >= qt * 4:
                        s_ = kt - qt * 4
                        op("pool", "tensor_tensor", out=pt[:, s_ * 128:(s_ + 1) * 128],
                           in0=pt[:, s_ * 128:(s_ + 1) * 128], in1=TRIB[:], op=ALU.mult)
                    pts.append((kt, pt))
                st_pts[i] = pts

            def back(i):
                h, qt, kb = steps[i]
                par = i % 2
                s_lo = 0 if kb <= 2 * qt else 2
                pts = st_pts.pop(i)
                acc = ACC[(h * NT + qt) % 2]
                pso = PS(4 + par)
                for s_ in range(s_lo, 4):
                    qs = qt * 4 + s_
                    rel = [(kt, pt) for (kt, pt) in pts if kt <= qs]
                    for j, (kt, pt) in enumerate(rel):
                        op("pe", "matmul", out=pso[:, s_ * 65:(s_ + 1) * 65],
                           lhsT=pt[:, s_ * 128:(s_ + 1) * 128], rhs=V[:, kt, h, :],
                           start=(j == 0), stop=(j == len(rel) - 1))
                for s_ in range(s_lo, 4):
                    qs = qt * 4 + s_
                    bq = qs // 2
                    gated = (bq >= 4 and kb < bq)
                    wsc = SEL[:, qs, h, kb:kb + 1] if gated else 1.0
                    if kb == 0:
                        op("dve", "tensor_scalar", out=acc[:, s_, :], in0=pso[:, s_ * 65:(s_ + 1) * 65],
                           scalar1=wsc, scalar2=None, op0=ALU.mult)
                    else:
                        op("dve", "scalar_tensor_tensor", out=acc[:, s_, :],
                           in0=pso[:, s_ * 65:(s_ + 1) * 65], scalar=wsc, in1=acc[:, s_, :],
                           op0=ALU.mult, op1=ALU.add)
                    if kb == bq:
                        op("dve", "reciprocal", out=RCP[:, s_:s_ + 1], in_=acc[:, s_, 64:65])
                        op("dve", "tensor_scalar", out=OT[:, qs, h * 64:(h + 1) * 64],
                           in0=acc[:, s_, 0:64], scalar1=RCP[:, s_:s_ + 1], scalar2=None, op0=ALU.mult)

            front(0)
            for i in range(len(steps)):
                if i + 1 < len(steps):
                    front(i + 1)
                back(i)
            P.phase = "m0.otT"
            for qt in range(NT):
                for c in range(4):
                    pb = (qt * 4 + c) % 2
                    pst = PS(pb).bitcast(BF16)
                    for s in range(4):
                        op("pe", "transpose", out=pst[:, s * 128:(s + 1) * 128],
                           in_=OT[:, qt * 4 + s, c * 128:(c + 1) * 128], identity=IDB[:])
                    evac(HB[:, 4 + c, qt * TT:(qt + 1) * TT], pst[:, 0:512])
            P.phase = "m0"
            out_proj(ab_w_out, lambda kc, ts: HB[:, kc, ts], 32 * KB_, after_tile)

        def mixer1(after_tile):
            WBI = [vw(i * 4 * KB_, [KC, 256], BF16) for i in range(2)]
            WG = vw(8 * KB_, [2, 8, 128], BF16)
            TMP = vw(12 * KB_, [1024], F32)
            GG = [vw(16 * KB_ + i * 4 * KB_, [T], BF16) for i in range(2)]
            XRB = [vw(24 * KB_ + i * 4608, [T + 4], BF16) for i in range(2)]
            HT = T // 2
            XC = [vw(33 * KB_ + q * 18 * KB_, [HT], F32) for q in range(2)]
            XCB = [vw(37 * KB_ + q * 18 * KB_, [HT], BF16) for q in range(2)]
            RA = [vw(39 * KB_ + q * 18 * KB_, [HT], F32) for q in range(2)]
            IH = [vw(43 * KB_ + q * 18 * KB_, [HT], F32) for q in range(2)]
            TB = [vw(47 * KB_ + q * 18 * KB_, [HT], F32) for q in range(2)]
            Y = vw(69 * KB_, [KC, T], BF16)
            DG4 = vw(101 * KB_, [4, 128], BF16)
            P.phase = "m1.chunks"
            for i, wsrc in enumerate((c_w_r, c_w_i)):
                op("pool", "dma_start", dma_key=("wg", i), out=WG[:, i, :, :],
                   in_=wsrc.rearrange("h i j -> i h j"))
            for i in range(2):
                op("dve", "memset", outs=("ap",), ap=XRB[i][:, 0:3], constant=0.0)

            def stage_a(c, hf):
                W = WBI[c % 2]
                gg = GG[c % 2]
                xrb = XRB[c % 2]
                if hf == 0:
                    op("sp", "dma_start", dma_key=("wbi", c % 2), out=W, in_=cw_scr[c], after=rl_inst[c])
                hs = slice(hf * 1024, (hf + 1) * 1024)
                for t2 in range(2):
                    tt = hf * 2 + t2
                    ts = slice(tt * TT, (tt + 1) * TT)
                    for kc in range(KC):
                        op("pe", "matmul", out=PS(t2), lhsT=W[:, kc, 0:128], rhs=HB[:, kc, ts],
                           start=(kc == 0), stop=(kc == KC - 1))
                    yield
                g2 = PSR(0, 2)
                op("act", "activation", out=TMP, in_=g2, func=AF.Square, scale=GELU_C ** 0.5)
                yield
                for t2 in range(2):
                    tt = hf * 2 + t2
                    ts = slice(tt * TT, (tt + 1) * TT)
                    for kc in range(KC):
                        op("pe", "matmul", out=PS(2 + t2), lhsT=W[:, kc, 128:256], rhs=HB[:, kc, ts],
                           start=(kc == 0), stop=(kc == KC - 1))
                    yield
                op("dve", "scalar_tensor_tensor", out=TMP, in0=TMP, scalar=1.0, in1=g2,
                   op0=ALU.add, op1=ALU.mult)
                yield
                op("act", "activation", out=xrb[:, 3 + hf * 1024: 3 + (hf + 1) * 1024], in_=PSR(2, 2),
                   func=AF.Copy)
                yield
                op("act", "activation", out=TMP, in_=TMP, func=AF.Tanh, scale=GELU_S)
                yield
                op("dve", "scalar_tensor_tensor", out=gg[:, hs], in0=TMP, scalar=1.0, in1=g2,
                   op0=ALU.add, op1=ALU.mult)
                yield

            ucount = [0]

            def stage_b(c, hf):
                u = ucount[0]
                ucount[0] += 1
                q = u % 2
                gg = GG[c % 2]
                xrb = XRB[c % 2]
                hs = slice(hf * HT, (hf + 1) * HT)
                xc, xcb, ra, ih, tb = XC[q], XCB[q], RA[q], IH[q], TB[q]
                if hf == 0:
                    for j in range(4):
                        col = V_CCW + j * 8 + c
                        op("dve", "tensor_scalar", out=DG4[:, j, :], in0=IDB[:],
                           scalar1=VEC[:, col:col + 1], scalar2=None, op0=ALU.mult)
                    yield
                for t2 in range(2):
                    tt = hf * 2 + t2
                    for j in range(4):
                        op("pe", "matmul", out=PS(4 + t2), lhsT=DG4[:, j, :],
                           rhs=xrb[:, tt * TT + j: tt * TT + j + TT], start=(j == 0), stop=(j == 3))
                yield
                op("act", "activation", out=xcb, in_=PSR(4, 2), func=AF.Identity,
                   bias=VEC[:, V_CCB + c:V_CCB + c + 1])
                yield
                op("act", "activation", out=xc, in_=PSR(4, 2), func=AF.Identity,
                   bias=VEC[:, V_CCB + c:V_CCB + c + 1])
                yield
                for gi, dstb in ((0, ra), (1, ih)):
                    for t2 in range(2):
                        op("pe", "matmul", out=PS(6 + t2), lhsT=WG[:, gi, c, :],
                           rhs=xcb[:, t2 * TT:(t2 + 1) * TT], start=True, stop=True)
                    yield
                    op("act", "activation", out=dstb, in_=PSR(6, 2), func=AF.Tanh, scale=0.5,
                       bias=HV[:, gi * 8 + c: gi * 8 + c + 1])
                    yield
                op("act", "activation", out=ra, in_=ra, func=AF.Exp, scale=HV[:, 16 + c:17 + c],
                   bias=HV[:, 16 + c:17 + c])
                yield
                op("dve", "tensor_tensor", out=tb, in0=ra, in1=ra, op=ALU.mult)
                yield
                op("dve", "tensor_scalar", out=tb, in0=tb, scalar1=1.0, scalar2=-1.0, op0=ALU.min, op1=ALU.mult)
                yield
                op("act", "activation", out=tb, in_=tb, func=AF.Sqrt, bias=1.0)
                yield
                op("dve", "scalar_tensor_tensor", out=tb, in0=ih, scalar=1.0, in1=tb, op0=ALU.add, op1=ALU.mult)
                yield
                op("dve", "scalar_tensor_tensor", out=tb, in0=tb, scalar=0.25, in1=xc, op0=ALU.mult, op1=ALU.mult)
                yield
                init = 0.0 if hf == 0 else IH[1 - q][:, HT - 1:HT]
                op("dve", "tensor_tensor_scan", out=ih, data0=ra, data1=tb, initial=init,
                   op0=ALU.mult, op1=ALU.add)
                yield
                op("dve", "tensor_tensor", out=Y[:, c, hs], in0=ih, in1=gg[:, hs], op=ALU.mult)
                yield

            def zip_run(*gens):
                gens = [g for g in gens if g is not None]
                while gens:
                    for g in list(gens):
                        try:
                            next(g)
                        except StopIteration:
                            gens.remove(g)

            zip_run(stage_a(0, 0))
            zip_run(stage_a(0, 1))
            for c in range(KC):
                if ZIP_M1:
                    for hf in range(2):
                        zip_run(stage_b(c, hf), stage_a(c + 1, hf) if c + 1 < KC else None)
                else:
                    if c + 1 < KC:
                        zip_run(stage_a(c + 1, 0))
                        zip_run(stage_a(c + 1, 1))
                    zip_run(stage_b(c, 0))
                    zip_run(stage_b(c, 1))
            P.phase = "m1"
            out_proj(c_w_out, lambda kc, ts: Y[:, kc, ts], [0, 12 * KB_], after_tile)

        def nothing(tt):
            pass

        for s in range(n_seq):
            nt0 = (lambda tt: norm_tile(V_MIX + 0, hb_dst, tt))
            nt1 = (lambda tt: norm_tile(V_MLP + 0, hb_dst, tt))
            nt2 = (lambda tt: norm_tile(V_MIX + 8, hb_dst, tt))
            nt3 = (lambda tt: norm_tile(V_MLP + 8, hb_dst, tt))
            load_x(s, nt0 if stop_stage >= 1 else nothing)
            if stop_stage >= 1:
                mixer0(nt1 if stop_stage >= 2 else nothing)
            if stop_stage >= 2:
                mlp(0, nt2 if stop_stage >= 3 else nothing)
            if stop_stage >= 3:
                mixer1(nt3 if stop_stage >= 4 else nothing)
            if stop_stage >= 4:
                mlp(1, (lambda tt, s=s: store_tile(s, tt, True, deferred=True)) if stop_stage >= 5 else nothing,
                    swap=True)
            if stop_stage < 5:
                for tt in range(NT):
                    store_tile(s, tt, False)
        P.emit(final_wait_keys=[("os", 0)])
        info = dict(n_inst=P.n, per_eng={e: len(P.insts[e]) for e in ENGS}, sig=P.sig_counts,
                    phases={e: [i.phase for i in P.insts[e]] for e in ENGS})
    return nc, info


def _feat(v):
    v = np.asarray(v, dtype=np.float32)
    return np.ascontiguousarray(v.reshape(-1, 128).T)


def make_vecs(inp):
    cols = np.zeros((128, NV), np.float32)
    for l in range(2):
        cols[:, V_MIX + l * 8:V_MIX + l * 8 + 8] = _feat(inp["mix_norm"][l])
        cols[:, V_MLP + l * 8:V_MLP + l * 8 + 8] = _feat(inp["mlp_norm"][l])
    cols[:, V_FIN:V_FIN + 8] = _feat(inp["final_norm"])
    cols[:, V_LNG:V_LNG + 4] = _feat(inp["ab_ln_g"][0])
    cols[:, V_LNB:V_LNB + 4] = _feat(inp["ab_ln_b"][0])
    for j in range(31):
        cols[:, V_CW + j * 4:V_CW + j * 4 + 4] = _feat(inp["ab_conv_w"][0][j])
    for j in range(4):
        cols[:, V_CCW + j * 8:V_CCW + j * 8 + 8] = _feat(inp["c_conv_w"][0][j])
    cols[:, V_CCB:V_CCB + 8] = _feat(inp["c_conv_b"][0])
    cols[:, V_BR:V_BR + 8] = _feat(np.asarray(inp["c_b_r"][0]).reshape(-1))
    cols[:, V_BI:V_BI + 8] = _feat(np.asarray(inp["c_b_i"][0]).reshape(-1))
    cols[:, V_LAM:V_LAM + 8] = _feat(inp["c_lambda"][0])
    return cols


def make_consts():
    c = np.zeros((128, NCST), np.float32)
    c[:, C_ID:C_ID + 128] = np.eye(128, dtype=np.float32)
    p = np.arange(128)
    for h in range(8):
        slope = 2.0 ** (-(h + 1))
        for i in range(20):
            c[:, C_BIAS + h * 20 + i] = slope * (p - 128.0 * (i - 3))
    c[:, C_M05] = -0.5
    c[:, C_P05] = 0.5
    return c


def make_tri():
    p = np.arange(128)
    return (p[:, None] <= p[None, :]).astype(np.float32)


_CACHE = {}


def run(inputs, n_cores=N_CORES, n_seq=SEQ_PER_CORE, stop_stage=5, seq_ids=None, trace=False):
    key = (n_seq, stop_stage)
    if key not in _CACHE:
        _CACHE[key] = build(n_seq, stop_stage)
    nc, info = _CACHE[key]
    f = lambda a: np.ascontiguousarray(np.asarray(a, dtype=np.float32))
    shared = {
        "vecs": make_vecs(inputs), "consts": make_consts(), "tri": make_tri(),
        "w_up": f(inputs["w_up"]), "w_down": f(inputs["w_down"]),
        "ab_w_in": f(inputs["ab_w_in"][0]), "ab_w_out": f(inputs["ab_w_out"][0]),
        "c_w_in": f(inputs["c_w_in"][0]), "c_w_out": f(inputs["c_w_out"][0]),
        "c_w_r": f(inputs["c_w_r"][0]), "c_w_i": f(inputs["c_w_i"][0]),
    }
    x = inputs["x"]
    in_maps = []
    for ci in range(n_cores):
        if seq_ids is None:
            xs = x[ci * n_seq:(ci + 1) * n_seq]
        else:
            xs = x[seq_ids[ci]]
        m = dict(shared)
        m["x"] = f(xs)
        in_maps.append(m)
    res = run_bass_kernel_spmd(nc, in_maps, core_ids=list(range(n_cores)), trace=trace)
    outs = [r["y"] for r in res.results]
    return np.concatenate(outs, axis=0), res, info


def kernel(**inputs):
    y, _, _ = run(inputs)
    return y.astype(np.float32)
```
